# Optimizing a Trainium2 kernel written in Bass

```python
import jax, jax.numpy as jnp
from jax import lax
import numpy as np

D_MODEL = 1024
BATCH = 32
SEQ = 2048
DEPTH = 1

RET_HEADS = 4
RET_HEAD_DIM = 128
RET_WIDTH = RET_HEADS * RET_HEAD_DIM
RET_CHUNK = 128
RET_ROPE_BASE = 10000.0
ATT_HEADS = 8
ATT_HEAD_DIM = 64
ATT_WIDTH = ATT_HEADS * ATT_HEAD_DIM
ATT_ROT_DIM = ATT_HEAD_DIM // 4
ATT_ROPE_BASE = 500000.0
MOBA_BLOCK = 256
MOBA_TOPK = 3
QUERY_BLOCK = 128
MIX_WIDTH = RET_WIDTH + ATT_WIDTH
IN_WIDTH = 4 * RET_WIDTH + 3 * ATT_WIDTH
FFN_DIM = 2816
NORM_EPS = 1e-6

kernel_name = "hybrid_retention_moba_macaron"


def _rms(x, w=None):
    xf = x.astype(jnp.float32)
    y = xf * lax.rsqrt(jnp.mean(xf * xf, axis=-1, keepdims=True) + NORM_EPS)
    if w is not None:
        y = y * w.astype(jnp.float32)
    return y.astype(x.dtype)


def _rope(x, theta, rot_dim):
    s = x.shape[1]
    half = rot_dim // 2
    inv_freq = theta ** (-jnp.arange(half, dtype=jnp.float32) * 2.0 / rot_dim)
    ang = jnp.arange(s, dtype=jnp.float32)[:, None] * inv_freq[None, :]
    cos = jnp.cos(ang)[None, :, None, :]
    sin = jnp.sin(ang)[None, :, None, :]
    xf = x.astype(jnp.float32)
    x1 = xf[..., :half]
    x2 = xf[..., half:rot_dim]
    out = jnp.concatenate([x1 * cos - x2 * sin, x2 * cos + x1 * sin, xf[..., rot_dim:]], axis=-1)
    return out.astype(x.dtype)


def _swiglu(h, w_gate, w_up, w_down):
    return (jax.nn.silu(h @ w_gate) * (h @ w_up)) @ w_down


def _retention(q, k, v):
    b, s, h, d = q.shape
    c = RET_CHUNK
    n = s // c
    log_g = jnp.log(1.0 - 2.0 ** (-5.0 - jnp.arange(h, dtype=jnp.float32)))
    i = jnp.arange(c, dtype=jnp.float32)
    diff = i[:, None] - i[None, :]
    inner_decay = jnp.where(diff[None] >= 0,
                            jnp.exp(jnp.maximum(diff, 0.0)[None] * log_g[:, None, None]), 0.0)
    q_decay = jnp.exp((i[None, :] + 1.0) * log_g[:, None])[None, :, :, None]
    k_decay = jnp.exp((c - 1.0 - i[None, :]) * log_g[:, None])[None, :, :, None]
    chunk_decay = jnp.exp(c * log_g)[None, :, None, None]

    def to_chunks(t):
        return t.reshape(b, n, c, h, d).transpose(1, 0, 3, 2, 4)

    def step(state, qkv):
        qc, kc, vc = qkv
        scores = jnp.einsum('bhid,bhjd->bhij', qc, kc) * inner_decay
        o = jnp.einsum('bhij,bhjd->bhid', scores, vc)
        o = o + jnp.einsum('bhid,bhde->bhie', qc, state) * q_decay
        state = state * chunk_decay + jnp.einsum('bhjd,bhje->bhde', kc * k_decay, vc)
        return state, o

    state0 = jnp.zeros((b, h, d, d), jnp.float32)
    _, o = lax.scan(step, state0, (to_chunks(q), to_chunks(k), to_chunks(v)))
    return o.transpose(1, 0, 3, 2, 4).reshape(b, s, h, d)


def _moba(q, k, v):
    b, s, h, dh = q.shape
    bs = MOBA_BLOCK
    qb = QUERY_BLOCK
    nb = -(-s // bs)
    pad = nb * bs - s
    nq = s // qb
    n_sel = min(MOBA_TOPK, nb)
    scale = dh ** -0.5
    qh = q.transpose(0, 2, 1, 3)
    kh = jnp.pad(k.transpose(0, 2, 1, 3), ((0, 0), (0, 0), (0, pad), (0, 0)))
    vh = jnp.pad(v.transpose(0, 2, 1, 3), ((0, 0), (0, 0), (0, pad), (0, 0)))
    k_blocks = kh.reshape(b, h, nb, bs, dh)
    v_blocks = vh.reshape(b, h, nb, bs, dh)
    k_mean = jnp.mean(k_blocks.astype(jnp.float32), axis=3)
    own = jnp.arange(s) // bs
    gate = jnp.einsum('bhsd,bhnd->bhsn', qh.astype(jnp.float32), k_mean)
    past = jnp.arange(nb)[None, :] < own[:, None]
    gate = jnp.where(past, gate, -jnp.inf)
    _, idx = lax.top_k(gate, n_sel)
    valid = idx < own[:, None]

    def blocks(t):
        t = t.reshape(b, h, nq, qb, *t.shape[3:])
        t = jnp.moveaxis(t, 2, 1)
        return t.reshape(b * nq, h, qb, *t.shape[4:])

    batch_id = jnp.repeat(jnp.arange(b), nq)
    qblk_id = jnp.tile(jnp.arange(nq), b)
    head_ix = jnp.arange(h)[:, None, None]

    def attend(args):
        qi, idx_i, valid_i, bi, ji = args
        kb = k_blocks[bi]
        vb = v_blocks[bi]
        k_sel = kb[head_ix, idx_i]
        v_sel = vb[head_ix, idx_i]
        own_blk = (ji * qb) // bs
        k_own = lax.dynamic_index_in_dim(kb, own_blk, axis=1, keepdims=False)
        v_own = lax.dynamic_index_in_dim(vb, own_blk, axis=1, keepdims=False)
        s_sel = jnp.einsum('hqd,hqnkd->hqnk', qi, k_sel).astype(jnp.float32) * scale
        s_sel = jnp.where(valid_i[..., None], s_sel, -jnp.inf)
        s_own = jnp.einsum('hqd,hkd->hqk', qi, k_own).astype(jnp.float32) * scale
        q_pos = ji * qb + jnp.arange(qb)
        k_pos = own_blk * bs + jnp.arange(bs)
        s_own = jnp.where(k_pos[None, None, :] <= q_pos[None, :, None], s_own, -jnp.inf)
        logits = jnp.concatenate([s_sel.reshape(h, qb, n_sel * bs), s_own], axis=-1)
        p = jax.nn.softmax(logits, axis=-1).astype(qi.dtype)
        p_sel = p[..., :n_sel * bs].reshape(h, qb, n_sel, bs)
        p_own = p[..., n_sel * bs:]
        return (jnp.einsum('hqnk,hqnkd->hqd', p_sel, v_sel)
                + jnp.einsum('hqk,hkd->hqd', p_own, v_own))

    out = lax.map(attend, (blocks(qh), blocks(idx), blocks(valid), batch_id, qblk_id))
    return out.reshape(b, nq, h, qb, dh).transpose(0, 1, 3, 2, 4).reshape(b, s, h * dh)


def setup_inputs(seed: int = 0) -> dict:
    key = jax.random.key(seed)
    ks = jax.random.split(key, 16)
    L = DEPTH

    def w(k, shape, fan_in):
        return jax.random.normal(k, shape, jnp.float32) * fan_in ** -0.5

    def gain(k, shape):
        return 1.0 + 0.02 * jax.random.normal(k, shape, jnp.float32)

    return {
        "x": jax.random.normal(ks[0], (BATCH, SEQ, D_MODEL), jnp.float32),
        "ffn1_norm_w": gain(ks[1], (L, D_MODEL)),
        "ffn1_w_gate": w(ks[2], (L, D_MODEL, FFN_DIM), D_MODEL),
        "ffn1_w_up": w(ks[3], (L, D_MODEL, FFN_DIM), D_MODEL),
        "ffn1_w_down": w(ks[4], (L, FFN_DIM, D_MODEL), FFN_DIM),
        "mix_norm_w": gain(ks[5], (L, D_MODEL)),
        "w_in": w(ks[6], (L, D_MODEL, IN_WIDTH), D_MODEL),
        "ret_out_beta": gain(ks[7], (L, RET_WIDTH)),
        "q_norm_w": gain(ks[8], (L, ATT_HEAD_DIM)),
        "k_norm_w": gain(ks[9], (L, ATT_HEAD_DIM)),
        "att_out_beta": gain(ks[10], (L, ATT_WIDTH)),
        "w_out": w(ks[11], (L, MIX_WIDTH, D_MODEL), MIX_WIDTH),
        "ffn2_norm_w": gain(ks[12], (L, D_MODEL)),
        "ffn2_w_gate": w(ks[13], (L, D_MODEL, FFN_DIM), D_MODEL),
        "ffn2_w_up": w(ks[14], (L, D_MODEL, FFN_DIM), D_MODEL),
        "ffn2_w_down": w(ks[15], (L, FFN_DIM, D_MODEL), FFN_DIM),
    }


def reference(x, ffn1_norm_w, ffn1_w_gate, ffn1_w_up, ffn1_w_down, mix_norm_w, w_in,
              ret_out_beta, q_norm_w, k_norm_w, att_out_beta, w_out,
              ffn2_norm_w, ffn2_w_gate, ffn2_w_up, ffn2_w_down):
    b, s, _ = x.shape
    R = RET_WIDTH
    A = ATT_WIDTH
    for l in range(DEPTH):
        x = x + 0.5 * _swiglu(_rms(x, ffn1_norm_w[l]), ffn1_w_gate[l], ffn1_w_up[l], ffn1_w_down[l])

        hn = _rms(x, mix_norm_w[l])
        proj = hn @ w_in[l]
        rq = proj[..., 0:R].reshape(b, s, RET_HEADS, RET_HEAD_DIM)
        rk = proj[..., R:2 * R].reshape(b, s, RET_HEADS, RET_HEAD_DIM)
        rv = proj[..., 2 * R:3 * R].reshape(b, s, RET_HEADS, RET_HEAD_DIM)
        rg = proj[..., 3 * R:4 * R]
        o0 = 4 * R
        aq = proj[..., o0:o0 + A].reshape(b, s, ATT_HEADS, ATT_HEAD_DIM)
        ak = proj[..., o0 + A:o0 + 2 * A].reshape(b, s, ATT_HEADS, ATT_HEAD_DIM)
        av = proj[..., o0 + 2 * A:o0 + 3 * A].reshape(b, s, ATT_HEADS, ATT_HEAD_DIM)

        rq = _rope(rq, RET_ROPE_BASE, RET_HEAD_DIM)
        rk = _rope(rk, RET_ROPE_BASE, RET_HEAD_DIM) * (RET_HEAD_DIM ** -0.5)
        ret = _retention(rq.astype(jnp.float32), rk.astype(jnp.float32), rv.astype(jnp.float32))
        ret = _rms(ret).reshape(b, s, R).astype(x.dtype)
        ret = ret * jax.nn.silu(rg) * ret_out_beta[l]

        aq = _rope(_rms(aq, q_norm_w[l]), ATT_ROPE_BASE, ATT_ROT_DIM)
        ak = _rope(_rms(ak, k_norm_w[l]), ATT_ROPE_BASE, ATT_ROT_DIM)
        att = _moba(aq, ak, av)
        att = _rms(att.reshape(b, s, ATT_HEADS, ATT_HEAD_DIM)).reshape(b, s, A) * att_out_beta[l]

        x = x + jnp.concatenate([ret, att], axis=-1) @ w_out[l]

        x = x + 0.5 * _swiglu(_rms(x, ffn2_norm_w[l]), ffn2_w_gate[l], ffn2_w_up[l], ffn2_w_down[l])
    return x
```

```python
import os
import numpy as np
import ml_dtypes
import concourse.bass as bass
import concourse.mybir as mybir
from concourse.bass_utils import run_bass_kernel_spmd

F32 = mybir.dt.float32
BF16 = mybir.dt.bfloat16
AF = mybir.ActivationFunctionType
ALU = mybir.AluOpType
AX = mybir.AxisListType

S = 2048
D = 1024
FF = 2816
NJ = 22
NT = 16
EPS = 1e-6
NEG = -30000.0
FENCE_ALL = bool(os.environ.get("K_FENCE_ALL"))


class Eng:
    def __init__(self, nc, e, name):
        self.nc, self.e, self.name = nc, e, name
        self.ep = 0
        self.sem = nc.alloc_semaphore(f"{name}_e0")
        self.cnt = 0
        self.waited = {}
        self.last_ins = None
        self.last_tok = None
        self.mysems = {self.sem.name}

    def new_epoch(self):
        self.ep += 1
        self.sem = self.nc.alloc_semaphore(f"{self.name}_e{self.ep}")
        self.mysems.add(self.sem.name)
        self.cnt = 0
        self.last_ins = None
        self.last_tok = None

    def __call__(self, ins):
        self.last_ins = ins
        self.last_tok = None
        if FENCE_ALL:
            sem, v = self.sig()
            self.e.wait_ge(sem, v)
        return ins

    def sig(self):
        if self.last_tok is None:
            assert self.last_ins is not None
            self.cnt += 1
            self.last_ins.then_inc(self.sem, 1)
            self.last_tok = (self.sem, self.cnt)
        return self.last_tok

    def fence(self):
        sem, v = self.sig()
        self.e.wait_ge(sem, v)

    def wait(self, *toks):
        for tok in toks:
            if tok is None:
                continue
            if isinstance(tok, (list, tuple)) and len(tok) and isinstance(tok[0], (list, tuple)):
                self.wait(*tok)
                continue
            sem, v = tok
            if sem.name in self.mysems:
                continue
            if self.waited.get(sem.name, 0) >= v:
                continue
            self.e.wait_ge(sem, v)
            self.waited[sem.name] = v


class DmaSlot:
    def __init__(self, nc, name):
        self.sem = nc.alloc_semaphore(name)
        self.cnt = 0

    def start(self, q, out, in_):
        q.e.dma_start(out=out, in_=in_).then_inc(self.sem, 16)
        self.cnt += 16
        return (self.sem, self.cnt)


class Queue:
    def __init__(self, e):
        self.e = e
        self.waited = {}

    def wait(self, *toks):
        for tok in toks:
            if tok is None:
                continue
            if isinstance(tok, (list, tuple)) and len(tok) and isinstance(tok[0], (list, tuple)):
                self.wait(*tok)
                continue
            sem, v = tok
            if self.waited.get(sem.name, 0) >= v:
                continue
            self.e.wait_ge(sem, v)
            self.waited[sem.name] = v


def _consts():
    c = {}
    bf = ml_dtypes.bfloat16
    c["ident_bf"] = np.eye(128, dtype=np.float32).astype(bf)
    c["ident_f"] = np.eye(128, dtype=np.float32)
    c["ones_bf"] = np.ones((128, 128), np.float32).astype(bf)
    pm = np.zeros((128, 128), np.float32)
    for dp in range(64):
        pm[dp + 64, dp] = -1.0
        pm[dp, dp + 64] = 1.0
    c["pm_bf"] = pm.astype(bf)
    pos = np.arange(S, dtype=np.float32)
    invf = (10000.0 ** (-np.arange(64, dtype=np.float32) * 2.0 / 128.0)).astype(np.float32)
    ang = pos[None, :] * invf[np.arange(128) % 64][:, None]
    c["cos_r"] = np.cos(ang).astype(np.float32).astype(bf)
    c["sin_r"] = np.sin(ang).astype(np.float32).astype(bf)
    g = 1.0 - 2.0 ** (-5.0 - np.arange(4, dtype=np.float64))
    i = np.arange(128, dtype=np.float64)
    qd = g[:, None] ** i[None, :]
    kd = (g[:, None] ** (-i[None, :])) * (128.0 ** -0.5)
    c["qdec"] = np.broadcast_to(qd[None].astype(np.float32), (128, 4, 128)).copy()
    c["kdec"] = np.broadcast_to(kd[None].astype(np.float32), (128, 4, 128)).copy()
    c["gC"] = [float(x) for x in (g ** 128)]
    jj = np.arange(128)
    c["maskT"] = (jj[:, None] <= jj[None, :]).astype(np.float32)
    c["cbT"] = np.where(jj[:, None] <= jj[None, :], 0.0, NEG).astype(np.float32).astype(bf)
    invfa = (500000.0 ** (-np.arange(8, dtype=np.float32) * 2.0 / 16.0)).astype(np.float32)
    p = np.arange(128)
    t = np.arange(NT)
    posa = (t[None, :] * 128 + p[:, None]).astype(np.float32)
    anga = posa[:, :, None] * invfa[None, None, :]
    c["cos_a"] = np.cos(anga).astype(np.float32)
    c["sin_a"] = np.sin(anga).astype(np.float32)
    e32 = np.zeros((64, 8, 128), np.float32)
    for n in range(8):
        e32[n, n, :] = 1.0
        e32[32 + n, n, :] = 1.0
    c["e32"] = e32.astype(bf)
    pmk = np.zeros((128, NT, 8), np.float32)
    for tt in range(NT):
        own = tt // 2
        pmk[:, tt, own:] = -1e30
    c["pastmask"] = pmk
    return c


CONST_SPECS = [
    ("ident_bf", [128, 128], BF16), ("ident_f", [128, 128], F32), ("ones_bf", [128, 128], BF16),
    ("pm_bf", [128, 128], BF16), ("cos_r", [128, S], BF16), ("sin_r", [128, S], BF16),
    ("qdec", [128, 4, 128], F32), ("kdec", [128, 4, 128], F32), ("maskT", [128, 128], F32),
    ("cbT", [128, 128], BF16), ("cos_a", [128, NT, 8], F32), ("sin_a", [128, NT, 8], F32),
    ("e32", [64, 8, 128], BF16), ("pastmask", [128, NT, 8], F32),
]

WSPECS = [
    ("ffn1_w_gate", [D, FF]), ("ffn1_w_up", [D, FF]), ("ffn1_w_down", [FF, D]),
    ("w_in", [D, 3584]), ("w_out", [D, D]),
    ("ffn2_w_gate", [D, FF]), ("ffn2_w_up", [D, FF]), ("ffn2_w_down", [FF, D]),
]
VSPECS = [("ffn1_norm_w", D), ("mix_norm_w", D), ("ffn2_norm_w", D), ("ret_out_beta", 512),
          ("att_out_beta", 512), ("q_norm_w", 64), ("k_norm_w", 64)]


def build(NSEQ=4, stop_after="all", do_cast=True, MIX_UNITS=None, DEBUG=False):
    C = _consts()
    nc = bass.Bass("TRN2", target_bir_lowering=False)
    x = nc.dram_tensor("x", [NSEQ, S, D], F32, kind="ExternalInput").ap()
    y = nc.dram_tensor("y", [NSEQ, S, D], F32, kind="ExternalOutput").ap()
    wf = {}
    wb = {}
    for name, shp in WSPECS:
        wf[name] = nc.dram_tensor(name, shp, F32, kind="ExternalInput").ap()
        wb[name] = nc.dram_tensor(name + "_bf", shp, BF16, kind="Internal").ap()
    vin = {}
    for name, n in VSPECS:
        vin[name] = nc.dram_tensor(name, [n], F32, kind="ExternalInput").ap()
    cd = {}
    for name, shp, dt in CONST_SPECS:
        cd[name] = nc.dram_tensor("c_" + name, shp, dt, kind="ExternalInput").ap()

    def sb(name, shp, dt):
        return nc.alloc_sbuf_tensor(name, shp, dt)

    dbg = {}
    if DEBUG:
        dbg["f"] = nc.dram_tensor("dbgf", [128, 16384], F32, kind="ExternalOutput").ap()
        dbg["b"] = nc.dram_tensor("dbgb", [128, 32768], BF16, kind="ExternalOutput").ap()
        dbg["sem"] = None
        dbg["toks"] = []

    def dump(ap2d, off, kind):
        if not DEBUG:
            return
        barrier()
        if dbg["sem"] is None:
            dbg["sem"] = DmaSlot(nc, "dbgsem")
        n = ap2d.shape[1]
        np_ = ap2d.shape[0]
        tok = dbg["sem"].start(SP, dbg[kind][0:np_, off:off + n], ap2d)
        SP.wait(tok)
        for E in ENGS:
            E.wait(tok)

    XT = sb("XT", [128, 8, S], F32)
    ARENA_N = 58432
    ARENA = sb("ARENA", [128, ARENA_N], BF16)
    cs = {}
    for name, shp, dt in CONST_SPECS:
        cs[name] = sb("k_" + name, shp, dt)
    nw = {k: sb("nw_" + k, [128, 8], F32) for k in ("ffn1_norm_w", "mix_norm_w", "ffn2_norm_w")}
    beta_r = sb("beta_r", [128, 4], F32)
    beta_a = sb("beta_a", [128, 4], F32)
    wqk = sb("wqk", [128, 4, 64], F32)
    RT = sb("RT", [128, 512], F32)
    RS = RT
    SG = [sb(f"SG{i}", [128, 512], BF16) for i in range(2)]
    QD = sb("QD", [128, 512], BF16)
    U32 = [sb(f"U32_{h}", [128, 128], F32) for h in range(1)]
    SBF = sb("SBF", [128, 128], BF16)
    SBFb = sb("SBFb", [128, 128], BF16)
    SCM = [sb(f"SCM{i}", [128, 128], BF16) for i in range(2)]
    ST = sb("ST", [128, 256], F32)
    GM = sb("GM", [128, NT, 2, 8], F32)
    TOP = sb("TOP", [128, NT, 2, 8], F32)
    ZB = sb("ZB", [128, 320], BF16)

    PSB = [nc.alloc_psum_tensor(f"PS{i}", [128, 512], F32) for i in range(8)]
    bank_free = [None] * 8

    PE = Eng(nc, nc.tensor, "pe")
    ACT = Eng(nc, nc.scalar, "act")
    DVE = Eng(nc, nc.vector, "dve")
    POOL = Eng(nc, nc.gpsimd, "pool")
    SP = Queue(nc.sync)
    ENGS = [PE, ACT, DVE, POOL]

    def ar(off, n):
        return ARENA[:, off:off + n]

    dsl = DmaSlot(nc, "cst")
    ctoks = []
    for name, shp, dt in CONST_SPECS:
        ctoks.append(dsl.start(SP, cs[name][:], cd[name]))
    with nc.allow_non_contiguous_dma(reason="tiny param layouts"):
        for k in nw:
            ctoks.append(dsl.start(SP, nw[k][:], vin[k].rearrange("(k p) -> p k", p=128)))
        ctoks.append(dsl.start(SP, beta_r[:], vin["ret_out_beta"].rearrange("(k p) -> p k", p=128)))
        ctoks.append(dsl.start(SP, beta_a[:], vin["att_out_beta"].rearrange("(k p) -> p k", p=128)))
        for g4, nm in enumerate(["q_norm_w", "q_norm_w", "k_norm_w", "k_norm_w"]):
            ctoks.append(dsl.start(SP, wqk[:, g4, :], vin[nm].partition_broadcast(128)))
    ctok = ctoks[-1]
    wtok = {}
    CH = 4096
    FST = [ARENA[:, i * 8192:(i + 1) * 8192].bitcast(F32) for i in range(2)]
    BST = [ARENA[:, 16384 + i * 4096:16384 + (i + 1) * 4096] for i in range(2)]
    ldr = [DmaSlot(nc, f"pl{i}") for i in range(2)]
    str_ = [DmaSlot(nc, f"ps{i}") for i in range(2)]
    f_free = [None, None]
    st_tok = [None, None]
    ci = 0
    cast_engs = None
    for name, shp in WSPECS:
        if not do_cast:
            wtok[name] = None
            continue
        R, Cc = shp
        n = R * Cc // 128
        src = wf[name].rearrange("r c -> (r c)").rearrange("(p n) -> p n", p=128)
        dst = wb[name].rearrange("r c -> (r c)").rearrange("(p n) -> p n", p=128)
        last = None
        for c0 in range(0, n, CH):
            c1 = min(n, c0 + CH)
            w_ = c1 - c0
            sl = ci % 2
            SP.wait(f_free[sl])
            ltok = ldr[sl].start(SP, FST[sl][:, 0:w_], src[:, c0:c1])
            E = [ACT, DVE, POOL][ci % 3]
            E.wait(ltok, st_tok[sl])
            if E is ACT:
                ACT(nc.scalar.copy(out=BST[sl][:, 0:w_], in_=FST[sl][:, 0:w_]))
            elif E is DVE:
                DVE(nc.vector.tensor_copy(out=BST[sl][:, 0:w_], in_=FST[sl][:, 0:w_]))
            else:
                POOL(nc.gpsimd.tensor_copy(out=BST[sl][:, 0:w_], in_=FST[sl][:, 0:w_]))
            ctk = E.sig()
            f_free[sl] = ctk
            SP.wait(ctk)
            st_tok[sl] = str_[sl].start(SP, dst[:, c0:c1], BST[sl][:, 0:w_])
            last = st_tok[sl]
            ci += 1
        wtok[name] = [t for t in st_tok if t is not None]
    for E in ENGS:
        E.wait(ctok)
    DVE(nc.vector.memset(ZB[:], 0.0))
    zb_tok = DVE.sig()
    PE.wait(zb_tok)
    SP.wait(*[t for t in st_tok if t is not None])

    def barrier():
        toks = [E.sig() for E in ENGS if E.last_ins is not None]
        for E in ENGS:
            E.wait(*toks)
        SP.wait(*toks)

    def pe_bank(b):
        PE.wait(bank_free[b])

    class Ring:
        def __init__(self, name, nslots):
            self.slots = [DmaSlot(nc, f"{name}{i}") for i in range(nslots)]
            self.free = [None] * nslots
            self.n = nslots
            self.i = 0

        def load(self, pairs, wait_tok=None):
            s = self.i % self.n
            self.i += 1
            SP.wait(self.free[s], wait_tok)
            tok = None
            for o, i_ in pairs:
                tok = self.slots[s].start(SP, o, i_)
            return s, tok

    xring = Ring("xr", 2)
    oring = Ring("or", 2)
    wgr = Ring("wg", 2)
    wdr = Ring("wd", 2)
    slr = Ring("sl", 2)
    wor = Ring("wo", 1)

    XST = [ARENA[:, 8192 + i * 2048: 8192 + (i + 1) * 2048].bitcast(F32) for i in range(2)]

    def stage_load(s):
        for t in range(NT):
            sl = xring.i % 2
            xring.i += 1
            SP.wait(xring.free[sl], oring.free[sl])
            tok = xring.slots[sl].start(SP, XST[sl], x[s, t * 128:(t + 1) * 128, :])
            PE.wait(tok)
            for hb in range(2):
                b = hb
                pe_bank(b)
                for k in range(4):
                    dc = hb * 4 + k
                    PE(nc.tensor.transpose(out=PSB[b][:, k * 128:(k + 1) * 128], in_=XST[sl][:, dc * 128:(dc + 1) * 128],
                                           identity=cs["ident_f"][:]))
                ptok = PE.sig()
                E = ACT if hb == 0 else DVE
                E.wait(ptok)
                src = PSB[b][:].rearrange("p (k c) -> p k c", k=4)
                dst = XT[:, hb * 4:(hb + 1) * 4, t * 128:(t + 1) * 128]
                if E is ACT:
                    ACT(nc.scalar.copy(out=dst, in_=src))
                else:
                    DVE(nc.vector.tensor_copy(out=dst, in_=src))
                bank_free[b] = E.sig()
            xring.free[sl] = ptok

    def stage_out(s):
        for t in range(NT):
            sl = oring.i % 2
            oring.i += 1
            for hb in range(2):
                b = hb
                pe_bank(b)
                for k in range(4):
                    dc = hb * 4 + k
                    PE(nc.tensor.transpose(out=PSB[b][:, k * 128:(k + 1) * 128], in_=XT[:, dc, t * 128:(t + 1) * 128],
                                           identity=cs["ident_f"][:]))
                ptok = PE.sig()
                E = ACT if hb == 0 else DVE
                E.wait(ptok, oring.free[sl])
                dst = XST[sl][:, hb * 512:(hb + 1) * 512]
                if E is ACT:
                    ACT(nc.scalar.copy(out=dst, in_=PSB[b][:]))
                else:
                    DVE(nc.vector.tensor_copy(out=dst, in_=PSB[b][:]))
                bank_free[b] = E.sig()
            SP.wait(bank_free[0], bank_free[1])
            oring.free[sl] = oring.slots[sl].start(SP, y[s, t * 128:(t + 1) * 128, :], XST[sl])

    def stage_norm(hT, t0, ntok, wname, SQoff):
        SQ = ARENA[:, SQoff:SQoff + 4096].rearrange("p (k c) -> p k c", k=8)
        w = nw[wname]
        for b0 in range(0, ntok, 512):
            ACT.wait(norm_state["sq_free"])
            for kc in range(8):
                ACT(nc.scalar.activation(out=SQ[:, kc, :], in_=XT[:, kc, t0 + b0:t0 + b0 + 512], func=AF.Square))
            sqtok = ACT.sig()
            PE.wait(sqtok)
            pe_bank(5)
            for kc in range(8):
                PE(nc.tensor.matmul(PSB[5][:], lhsT=cs["ones_bf"][:], rhs=SQ[:, kc, :], start=(kc == 0), stop=(kc == 7)))
            ptok = PE.sig()
            norm_state["sq_free"] = ptok
            ACT.wait(ptok, norm_state["rt_free"])
            ACT(nc.scalar.activation(out=RT[:], in_=PSB[5][:], func=AF.Sqrt, bias=epsb[:, 0:1], scale=1.0 / D))
            rttok = ACT.sig()
            bank_free[5] = rttok
            DVE.wait(rttok)
            DVE(nc.vector.reciprocal(out=RS[:], in_=RT[:]))
            for kc in range(8):
                DVE(nc.vector.scalar_tensor_tensor(out=hT[:, kc, b0:b0 + 512], in0=XT[:, kc, t0 + b0:t0 + b0 + 512],
                                                   scalar=w[:, kc:kc + 1], in1=RS[:], op0=ALU.mult, op1=ALU.mult))
            norm_state["rt_free"] = DVE.sig()
        return DVE.sig()

    norm_state = {"sq_free": None, "rt_free": None}
    epsb = sb("epsb", [128, 1], F32)
    DVE(nc.vector.memset(epsb[:], EPS))
    ACT.wait(DVE.sig())

    def stage_ffn(which):
        wg, wu, wd = wb[f"{which}_w_gate"], wb[f"{which}_w_up"], wb[f"{which}_w_down"]
        wgv = wg.rearrange("(kc p) f -> p kc f", p=128)
        wuv = wu.rearrange("(kc p) f -> p kc f", p=128)
        wdv = wd.rearrange("(j p) d -> p j d", p=128)
        wt = [wtok[f"{which}_w_gate"], wtok[f"{which}_w_up"], wtok[f"{which}_w_down"]]
        hT = ar(0, 8192).rearrange("p (k c) -> p k c", k=8)
        actT = ar(8192, 22528).rearrange("p (j c) -> p j c", j=NJ)
        WG = [ar(30720 + i * 2048, 2048).rearrange("p (k c) -> p k c", k=8) for i in range(2)]
        WU = [ar(34816 + i * 2048, 2048).rearrange("p (k c) -> p k c", k=8) for i in range(2)]
        WD = [ar(38912 + i * 5632, 5632).rearrange("p (j c) -> p j c", j=NJ) for i in range(2)]
        NSL = 11
        for G in range(2):
            t0 = G * 1024

            def load_gu(i):
                c0 = i * 256
                s_ = wgr.i % 2
                wgr.i += 1
                SP.wait(wgr.free[s_], wt[0], wt[1])
                wgr.slots[s_].start(SP, WG[s_][:], wgv[:, :, c0:c0 + 256])
                tok = wgr.slots[s_].start(SP, WU[s_][:], wuv[:, :, c0:c0 + 256])
                return s_, tok

            def load_d(i):
                c0 = i * 256
                s_ = wdr.i % 2
                wdr.i += 1
                SP.wait(wdr.free[s_], wt[2])
                tok = wdr.slots[s_].start(SP, WD[s_][:], wdv[:, :, c0:c0 + 256])
                return s_, tok

            pend = load_gu(0)
            pendd = load_d(0)
            htok = stage_norm(hT, t0, 1024, f"{which}_norm_w", 50176)
            PE.wait(htok)
            gi = 0
            for i in range(NSL):
                cur = pend
                if i + 1 < NSL:
                    pend = load_gu(i + 1)
                s_, tok = cur
                PE.wait(tok)
                for jj in range(2):
                    j = i * 2 + jj
                    for th in range(2):
                        bg = (gi % 2) * 2
                        bu = bg + 1
                        gi += 1
                        pe_bank(bg)
                        for kc in range(8):
                            PE(nc.tensor.matmul(PSB[bg][:], lhsT=WG[s_][:, kc, jj * 128:(jj + 1) * 128],
                                                rhs=hT[:, kc, th * 512:(th + 1) * 512], start=(kc == 0), stop=(kc == 7)))
                        gtok = PE.sig()
                        pe_bank(bu)
                        for kc in range(8):
                            PE(nc.tensor.matmul(PSB[bu][:], lhsT=WU[s_][:, kc, jj * 128:(jj + 1) * 128],
                                                rhs=hT[:, kc, th * 512:(th + 1) * 512], start=(kc == 0), stop=(kc == 7)))
                        utok = PE.sig()
                        sg = SG[(gi) % 2]
                        ACT.wait(gtok, ffn_state["sg_free"][gi % 2])
                        ACT(nc.scalar.activation(out=sg[:], in_=PSB[bg][:], func=AF.Silu))
                        stok = ACT.sig()
                        bank_free[bg] = stok
                        DVE.wait(stok, utok, ffn_state["act_free"])
                        DVE(nc.vector.tensor_tensor(out=actT[:, j, th * 512:(th + 1) * 512], in0=PSB[bu][:], in1=sg[:], op=ALU.mult))
                        dtok = DVE.sig()
                        bank_free[bu] = dtok
                        ffn_state["sg_free"][gi % 2] = dtok
                wgr.free[s_] = PE.sig()
            hT_free = PE.sig()
            acttok = DVE.sig()
            PE.wait(acttok)
            oi = 0
            for i in range(4):
                cur = pendd
                if i + 1 < 4:
                    pendd = load_d(i + 1)
                s_, tok = cur
                PE.wait(tok)
                for dl in range(2):
                    dc = i * 2 + dl
                    for th in range(2):
                        b = 4 + (oi % 2)
                        oi += 1
                        pe_bank(b)
                        for j in range(NJ):
                            PE(nc.tensor.matmul(PSB[b][:], lhsT=WD[s_][:, j, dl * 128:(dl + 1) * 128],
                                                rhs=actT[:, j, th * 512:(th + 1) * 512], start=(j == 0), stop=(j == NJ - 1)))
                        otok = PE.sig()
                        DVE.wait(otok)
                        xs = XT[:, dc, t0 + th * 512:t0 + (th + 1) * 512]
                        DVE(nc.vector.scalar_tensor_tensor(out=xs, in0=PSB[b][:], scalar=0.5, in1=xs, op0=ALU.mult, op1=ALU.add))
                        bank_free[b] = DVE.sig()
                wdr.free[s_] = PE.sig()
            ffn_state["act_free"] = PE.sig()
            DVE.wait(hT_free)
            ACT.wait(DVE.sig())

    ffn_state = {"sg_free": [None, None], "act_free": None}

    def stage_mix():
        hT2 = ar(0, 16384).rearrange("p (k c) -> p k c", k=8)
        mixT = ar(16384, 16384).rearrange("p (k c) -> p k c", k=8)
        SLAB = [ar(32768 + i * 4096, 4096) for i in range(2)]
        U0 = 40960
        qT = ar(U0, 2048)
        kT = ar(U0 + 2048, 2048)
        vtok = ar(U0 + 4096, 2048).rearrange("p (t c) -> p t c", t=NT)
        gs = ar(U0 + 6144, 2048)
        ktok = ar(U0 + 8192, 2048).rearrange("p (t c) -> p t c", t=NT)
        QKt = ar(U0, 4096).rearrange("p (t g c) -> p t g c", t=NT, g=4)
        ATTt = ar(U0, 2048).rearrange("p (t c) -> p t c", t=NT)
        PT = [ar(U0 + 2048 + i * 512, 512) for i in range(4)]
        vext = ar(U0 + 4096, 2080).rearrange("p (t h c) -> p t h c", t=NT, h=2)
        qTp = ar(U0 + 6208, 2048)
        kTp = ar(U0 + 8256, 2048)
        biasT = ar(U0 + 10304, 2048)
        BIASt = ar(U0 + 12352, 1024).rearrange("p (t h c) -> p t h c", t=NT, h=2)
        WO = ar(U0, 8192).rearrange("p (k c) -> p k c", k=8)
        SQo = 54336
        TA = ar(SQo, 2048).bitcast(F32)
        T1 = TA[:, 0:512]
        T2 = TA[:, 512:1024]
        SQ1 = ar(SQo + 2048, 512)
        TB = ar(SQo + 2560, 1024).bitcast(F32)
        winv = wb["w_in"].rearrange("(kc p) c -> p kc c", p=128)
        B7 = PSB[7][:].bitcast(BF16)
        B6 = PSB[6][:].bitcast(BF16)
        gCs = C["gC"]

        def load_slab(u):
            s_ = slr.i % 2
            slr.i += 1
            SP.wait(slr.free[s_], wtok["w_in"])
            tok = None
            if u < 4:
                v = SLAB[s_].rearrange("p (k a c) -> p k a c", k=8, a=4)
                for part in range(4):
                    c0 = part * 512 + u * 128
                    tok = slr.slots[s_].start(SP, v[:, :, part, :], winv[:, :, c0:c0 + 128])
            else:
                v = SLAB[s_][:, 0:3072].rearrange("p (k a c) -> p k a c", k=8, a=3)
                for part in range(3):
                    c0 = 2048 + part * 512 + (u - 4) * 128
                    tok = slr.slots[s_].start(SP, v[:, :, part, :], winv[:, :, c0:c0 + 128])
            return s_, v, tok

        units = list(range(8)) if MIX_UNITS is None else MIX_UNITS
        with nc.allow_non_contiguous_dma(reason="256B weight rows"):
            pend = load_slab(units[0])
        htok = stage_norm(hT2, 0, S, "mix_norm_w", SQo)
        PE.wait(htok)
        ACT.wait(htok)
        st = {"scm_free": [None, None], "pt_free": [None] * 4, "sbf_tok": None, "sbf_free": None}

        def ret_unit(h, slab, stok):
            PE.wait(stok)
            bi = 0
            for g in range(4):
                blk = slice(g * 512, (g + 1) * 512)
                for part, dec, dstT in ((0, cs["qdec"], qT), (1, cs["kdec"], kT)):
                    bq = bi % 2
                    br = 2 + bi % 2
                    bi += 1
                    pe_bank(bq)
                    for kc in range(8):
                        PE(nc.tensor.matmul(PSB[bq][:], lhsT=slab[:, kc, part, :], rhs=hT2[:, kc, blk], start=(kc == 0), stop=(kc == 7)))
                    ptok = PE.sig()
                    DVE.wait(ptok)
                    DVE(nc.vector.tensor_tensor(out=QD[:].rearrange("p (a c) -> p a c", a=4),
                                                in0=PSB[bq][:].rearrange("p (a c) -> p a c", a=4),
                                                in1=dec[:, h, :].unsqueeze(1).broadcast_to([128, 4, 128]), op=ALU.mult))
                    qdtok = DVE.sig()
                    bank_free[bq] = qdtok
                    PE.wait(qdtok)
                    pe_bank(br)
                    PE(nc.tensor.matmul(PSB[br][:], lhsT=cs["pm_bf"][:], rhs=QD[:], start=True, stop=True))
                    rtok = PE.sig()
                    DVE(nc.vector.tensor_tensor(out=T1, in0=QD[:], in1=cs["cos_r"][:, blk], op=ALU.mult))
                    DVE.wait(rtok)
                    DVE(nc.vector.tensor_tensor(out=T2, in0=PSB[br][:], in1=cs["sin_r"][:, blk], op=ALU.mult))
                    bank_free[br] = DVE.sig()
                    DVE(nc.vector.tensor_tensor(out=dstT[:, blk], in0=T1, in1=T2, op=ALU.add))
                bq = bi % 2
                bi += 1
                pe_bank(bq)
                for kc in range(8):
                    PE(nc.tensor.matmul(PSB[bq][:], lhsT=slab[:, kc, 3, :], rhs=hT2[:, kc, blk], start=(kc == 0), stop=(kc == 7)))
                ptok = PE.sig()
                ACT.wait(ptok)
                ACT(nc.scalar.activation(out=gs[:, blk], in_=PSB[bq][:], func=AF.Silu))
                bank_free[bq] = ACT.sig()
                bq = bi % 2
                bi += 1
                pe_bank(bq)
                for tl in range(4):
                    t = g * 4 + tl
                    for kc in range(8):
                        PE(nc.tensor.matmul(PSB[bq][:, tl * 128:(tl + 1) * 128], lhsT=hT2[:, kc, t * 128:(t + 1) * 128],
                                            rhs=slab[:, kc, 2, :], start=(kc == 0), stop=(kc == 7)))
                ptok = PE.sig()
                ACT.wait(ptok)
                ACT(nc.scalar.copy(out=vtok[:, g * 4:(g + 1) * 4, :], in_=PSB[bq][:].rearrange("p (t c) -> p t c", t=4)))
                bank_free[bq] = ACT.sig()
            slab_done = PE.sig()
            qk_tok_ = DVE.sig()
            v_tok_ = ACT.sig()
            PE.wait(qk_tok_, v_tok_)
            for half in range(2):
                pe_bank(7)
                for c in range(8):
                    n = half * 8 + c
                    PE(nc.tensor.transpose(out=B7[:, c * 128:(c + 1) * 128], in_=kT[:, n * 128:(n + 1) * 128], identity=cs["ident_bf"][:]))
                ptok = PE.sig()
                ACT.wait(ptok)
                ACT(nc.scalar.copy(out=ktok[:, half * 8:(half + 1) * 8, :], in_=B7.rearrange("p (t c) -> p t c", t=8)))
                bank_free[7] = ACT.sig()
            PE.wait(ACT.sig())
            U = U32[0]
            SBF2 = [SBF, SBFb]
            toks = {}

            def o_part(n):
                ch = slice(n * 128, (n + 1) * 128)
                bo = 4 + (n // 4) % 2
                col = (n % 4) * 128
                PE.wait(toks["scm", n])
                if n % 4 == 0:
                    pe_bank(bo)
                PE(nc.tensor.matmul(PSB[bo][:, col:col + 128], lhsT=vtok[:, n, :], rhs=SCM[n % 2][:], start=True, stop=(n == 0)))
                if n > 0:
                    PE.wait(toks["sbf", n])
                    PE(nc.tensor.matmul(PSB[bo][:, col:col + 128], lhsT=SBF2[n % 2][:], rhs=qT[:, ch], start=False, stop=True))
                otok = PE.sig()
                toks["o", n] = otok
                st["scm_free"][n % 2] = otok

            def epi(n):
                bo = 4 + (n // 4) % 2
                blk = slice((n // 4) * 512, (n // 4 + 1) * 512)
                ACT.wait(toks["o", n])
                ACT(nc.scalar.activation(out=SQ1, in_=PSB[bo][:], func=AF.Square))
                PE.wait(ACT.sig())
                pe_bank(6)
                PE(nc.tensor.matmul(PSB[6][:], lhsT=cs["ones_bf"][:], rhs=SQ1, start=True, stop=True))
                ACT.wait(PE.sig())
                ACT(nc.scalar.activation(out=RT[:], in_=PSB[6][:], func=AF.Sqrt, bias=epsb[:, 0:1], scale=1.0 / 128))
                rt_ = ACT.sig()
                bank_free[6] = rt_
                DVE.wait(rt_)
                DVE(nc.vector.reciprocal(out=RS[:], in_=RT[:]))
                DVE(nc.vector.tensor_tensor(out=T1, in0=PSB[bo][:], in1=RS[:], op=ALU.mult))
                bank_free[bo] = DVE.sig()
                DVE(nc.vector.scalar_tensor_tensor(out=mixT[:, h, blk], in0=T1, scalar=beta_r[:, h:h + 1], in1=gs[:, blk],
                                                   op0=ALU.mult, op1=ALU.mult))
                ACT.wait(DVE.sig())

            for n in range(NT + 1):
                if n < NT:
                    ch = slice(n * 128, (n + 1) * 128)
                    bs = n % 2
                    bk = 2 + n % 2
                    pe_bank(bs)
                    PE(nc.tensor.matmul(PSB[bs][:, 0:128], lhsT=kT[:, ch], rhs=qT[:, ch], start=True, stop=True))
                    sctok = PE.sig()
                    if n < NT - 1:
                        pe_bank(bk)
                        PE(nc.tensor.matmul(PSB[bk][:, 0:128], lhsT=ktok[:, n, :], rhs=vtok[:, n, :], start=True, stop=True))
                        kvtok = PE.sig()
                    DVE.wait(sctok, st["scm_free"][n % 2])
                    DVE(nc.vector.tensor_tensor(out=SCM[n % 2][:], in0=PSB[bs][:, 0:128], in1=cs["maskT"][:], op=ALU.mult))
                    toks["scm", n] = DVE.sig()
                    bank_free[bs] = toks["scm", n]
                if n >= 1:
                    o_part(n - 1)
                if n < NT - 1:
                    DVE.wait(kvtok, toks.get(("sbf", n)))
                    if n == 0:
                        DVE(nc.vector.tensor_copy(out=U[:], in_=PSB[bk][:, 0:128]))
                    else:
                        DVE(nc.vector.scalar_tensor_tensor(out=U[:], in0=U[:], scalar=gCs[h], in1=PSB[bk][:, 0:128], op0=ALU.mult, op1=ALU.add))
                    utok = DVE.sig()
                    bank_free[bk] = utok
                    ACT.wait(utok, toks.get(("o", n - 1)))
                    ACT(nc.scalar.activation(out=SBF2[(n + 1) % 2][:], in_=U[:], func=AF.Copy, scale=gCs[h]))
                    toks["sbf", n + 1] = ACT.sig()
                if n >= 1 and (n - 1) % 4 == 3:
                    epi(n - 1)
            return slab_done

        def att_unit(p, slab, stok):
            PE.wait(stok)
            slab2 = slab.rearrange("p k a c -> p k (a c)")
            DVE(nc.vector.memset(vext[:, :, :, 64:65], 1.0))
            DVE(nc.vector.memset(BIASt[:].rearrange("p t h c -> p (t h c)"), 0.0))
            init_tok = DVE.sig()
            ACT.wait(init_tok)
            for t in range(NT):
                b = t % 2
                pe_bank(b)
                for kc in range(8):
                    PE(nc.tensor.matmul(PSB[b][:, 0:384], lhsT=hT2[:, kc, t * 128:(t + 1) * 128], rhs=slab2[:, kc, :], start=(kc == 0), stop=(kc == 7)))
                ptok = PE.sig()
                ACT.wait(ptok)
                ACT(nc.scalar.copy(out=QKt[:, t, :, :], in_=PSB[b][:, 0:256].rearrange("p (g c) -> p g c", g=4)))
                ACT(nc.scalar.copy(out=vext[:, t, :, 0:64], in_=PSB[b][:, 256:384].rearrange("p (h c) -> p h c", h=2)))
                bank_free[b] = ACT.sig()
            slab_done = PE.sig()
            raw_tok = ACT.sig()
            DVE.wait(raw_tok)
            SS = ST[:, 0:64]
            TAv = TA.rearrange("p (t g c) -> p t g c", t=4, g=4)
            ACT.wait(DVE.sig())
            for t in range(NT):
                for gg in range(4):
                    ix = t * 4 + gg
                    ACT(nc.scalar.activation(out=TA[:, 0:64], in_=QKt[:, t, gg, :], func=AF.Square, accum_out=SS[:, ix:ix + 1]))
            ACT(nc.scalar.activation(out=ST[:, 128:192], in_=SS, func=AF.Sqrt, bias=epsb[:, 0:1], scale=1.0 / 64))
            DVE.wait(ACT.sig())
            DVE(nc.vector.reciprocal(out=SS, in_=ST[:, 128:192]))
            DVE.fence()
            RP = TB.rearrange("p (a t g c) -> p a t g c", a=4, t=4, g=4)
            for q4 in range(4):
                src = QKt[:, q4 * 4:(q4 + 1) * 4, :, :]
                rsb = SS[:, q4 * 16:(q4 + 1) * 16].rearrange("p (t g) -> p t g", t=4).unsqueeze(3).broadcast_to([128, 4, 4, 64])
                for tt in range(4):
                    for gg in range(4):
                        ix = q4 * 16 + tt * 4 + gg
                        DVE(nc.vector.tensor_scalar(out=TAv[:, tt, gg, :], in0=src[:, tt, gg, :], scalar1=SS[:, ix:ix + 1], scalar2=None, op0=ALU.mult))
                DVE(nc.vector.tensor_tensor(out=TAv, in0=TAv, in1=wqk[:].unsqueeze(1).broadcast_to([128, 4, 4, 64]), op=ALU.mult))
                cosb = cs["cos_a"][:, q4 * 4:(q4 + 1) * 4, :].unsqueeze(2).broadcast_to([128, 4, 4, 8])
                sinb = cs["sin_a"][:, q4 * 4:(q4 + 1) * 4, :].unsqueeze(2).broadcast_to([128, 4, 4, 8])
                x1 = TAv[:, :, :, 0:8]
                x2 = TAv[:, :, :, 8:16]
                DVE(nc.vector.tensor_tensor(out=RP[:, 0], in0=x1, in1=cosb, op=ALU.mult))
                DVE(nc.vector.tensor_tensor(out=RP[:, 1], in0=x2, in1=sinb, op=ALU.mult))
                DVE(nc.vector.tensor_tensor(out=RP[:, 2], in0=x2, in1=cosb, op=ALU.mult))
                DVE(nc.vector.tensor_tensor(out=RP[:, 3], in0=x1, in1=sinb, op=ALU.mult))
                DVE(nc.vector.tensor_tensor(out=src[:, :, :, 0:8], in0=RP[:, 0], in1=RP[:, 1], op=ALU.subtract))
                DVE(nc.vector.tensor_tensor(out=src[:, :, :, 8:16], in0=RP[:, 2], in1=RP[:, 3], op=ALU.add))
                DVE(nc.vector.tensor_copy(out=src[:, :, :, 16:64], in_=TAv[:, :, :, 16:64]))
            if p == 0:
                dump(ar(U0, 4096), 0, "b")
                dump(SS, 512, "f")
            PE.wait(DVE.sig())
            for which, dst, gsl in (("k", kTp, slice(2, 4)), ("q", qTp, slice(0, 2))):
                for half in range(2):
                    pe_bank(7)
                    for c in range(8):
                        t = half * 8 + c
                        PE(nc.tensor.transpose(out=B7[:, c * 128:(c + 1) * 128], in_=QKt[:, t, gsl, :].rearrange("p g c -> p (g c)"),
                                               identity=cs["ident_bf"][:]))
                    ptok = PE.sig()
                    ACT.wait(ptok)
                    ACT(nc.scalar.copy(out=dst[:, half * 1024:(half + 1) * 1024], in_=B7))
                    bank_free[7] = ACT.sig()
            qk_dead = PE.sig()
            tr_tok = ACT.sig()
            DVE.wait(tr_tok, qk_dead)
            KS = SCM[0][:, 0:8]
            for nb in range(8):
                ACT(nc.scalar.activation(out=TA[:, 0:256], in_=kTp[:, nb * 256:(nb + 1) * 256], func=AF.Copy, accum_out=ST[:, 16 + nb:17 + nb]))
            ACT(nc.scalar.copy(out=ST[:, 32:40], in_=ST[:, 16:24]))
            DVE.wait(ACT.sig())
            DVE(nc.vector.tensor_copy(out=KS, in_=ST[:, 32:40]))
            PE.wait(DVE.sig(), tr_tok)
            pe_bank(2)
            for t in range(NT):
                for hl in range(2):
                    o_ = (t * 2 + hl) * 8
                    PE(nc.tensor.matmul(PSB[2][:, o_:o_ + 8], lhsT=qTp[hl * 64:(hl + 1) * 64, t * 128:(t + 1) * 128],
                                        rhs=KS[hl * 64:(hl + 1) * 64, :], start=True, stop=True))
            DVE.wait(PE.sig())
            DVE(nc.vector.tensor_tensor(out=GM[:], in0=PSB[2][:, 0:256].rearrange("p (t h n) -> p t h n", t=NT, h=2),
                                        in1=cs["pastmask"][:].unsqueeze(2).broadcast_to([128, NT, 2, 8]), op=ALU.add))
            bank_free[2] = DVE.sig()
            for t in range(NT):
                for hl in range(2):
                    DVE(nc.vector.max(out=TOP[:, t, hl, :], in_=GM[:, t, hl, :]))
            for t in range(NT):
                for hl in range(2):
                    DVE(nc.vector.tensor_tensor(out=GM[:, t, hl, :], in0=GM[:, t, hl, :], in1=TOP[:, t, hl, 2:3].broadcast_to([128, 8]), op=ALU.is_lt))
            DVE(nc.vector.tensor_scalar(out=BIASt[:, :, :, 0:8], in0=GM[:], scalar1=NEG, scalar2=None, op0=ALU.mult))
            PE.wait(DVE.sig())
            for half in range(2):
                pe_bank(7)
                for c in range(8):
                    t = half * 8 + c
                    PE(nc.tensor.transpose(out=B7[0:64, c * 128:(c + 1) * 128], in_=BIASt[:, t, :, :].rearrange("p h c -> p (h c)"),
                                           identity=cs["ident_bf"][:]))
                ptok = PE.sig()
                ACT.wait(ptok)
                ACT(nc.scalar.copy(out=biasT[0:64, half * 1024:(half + 1) * 1024], in_=B7[0:64, :]))
                bank_free[7] = ACT.sig()
            PE.wait(ACT.sig())
            if p == 0:
                dump(qTp, 4096, "b")
                dump(kTp, 6144, "b")
                dump(GM[:].rearrange("p t h n -> p (t h n)"), 0, "f")
                dump(TOP[:].rearrange("p t h n -> p (t h n)"), 256, "f")
                dump(ar(U0 + 12352, 1024), 8192, "b")
                dump(biasT[0:64, :], 9216, "b")
                dump(ar(U0 + 4096, 2080), 13312, "b")
            steps = []
            for hl in range(2):
                for G in range(4):
                    for kt in range(4 * G + 4):
                        steps.append((hl, G, kt))
            ST_ = {}
            RI = ST[:, 0:4]
            S4 = ST[:, 8:12]
            S4b = ST[:, 192:196]
            S4c = ST[:, 200:204]
            ON2 = [TA[:, i * 512:i * 512 + 256].rearrange("p (q c) -> p q c", q=4) for i in range(2)]
            SQn2 = [TA[:, i * 512 + 256:i * 512 + 512].rearrange("p (q c) -> p q c", q=4) for i in range(2)]

            def emit_scores(i):
                hl, G, kt = steps[i]
                hp = slice(hl * 64, (hl + 1) * 64)
                bp = slice(hl * 32, (hl + 1) * 32)
                gi = hl * 4 + G
                bo = 4 + gi % 2
                if kt == 0:
                    pe_bank(bo)
                    PE(nc.tensor.matmul(PSB[bo][:, 0:260], lhsT=ZB[:, 0:128], rhs=ZB[:, 0:260], start=True, stop=False, skip_group_check=True))
                r = kt - 4 * G
                q0 = max(r, 0) * 128
                nblk = kt // 2
                if nblk < 2 * G:
                    b0 = q0
                elif nblk == 2 * G:
                    b0 = 256
                else:
                    b0 = None
                causal = r >= 0
                bs = i % 2
                pe_bank(bs)
                PE(nc.tensor.matmul(PSB[bs][:, q0:512], lhsT=kTp[hp, kt * 128:(kt + 1) * 128], rhs=qTp[hp, G * 512 + q0:(G + 1) * 512],
                                    start=True, stop=(b0 is None and not causal)))
                if b0 is not None:
                    PE(nc.tensor.matmul(PSB[bs][:, b0:512], lhsT=cs["e32"][bp, nblk, :], rhs=biasT[bp, G * 512 + b0:(G + 1) * 512],
                                        start=False, stop=(not causal)))
                if causal:
                    PE(nc.tensor.matmul(PSB[bs][:, r * 128:(r + 1) * 128], lhsT=cs["ident_bf"][:], rhs=cs["cbT"][:], start=False, stop=True))
                sctok = PE.sig()
                slot = i % 4
                ACT.wait(sctok, st["pt_free"][slot])
                ACT(nc.scalar.activation(out=PT[slot][:, q0:512], in_=PSB[bs][:, q0:512], func=AF.Exp, scale=0.125))
                etok = ACT.sig()
                bank_free[bs] = etok
                ST_[i] = etok

            def emit_pv(i):
                hl, G, kt = steps[i]
                gi = hl * 4 + G
                bo = 4 + gi % 2
                r = kt - 4 * G
                slot = i % 4
                PE.wait(ST_[i])
                for qtl in range(max(r, 0), 4):
                    PE(nc.tensor.matmul(PSB[bo][:, qtl * 65:qtl * 65 + 65], lhsT=PT[slot][:, qtl * 128:(qtl + 1) * 128],
                                        rhs=vext[:, kt, hl, :], start=False, stop=(kt == 4 * G + qtl), skip_group_check=True))
                st["pt_free"][slot] = PE.sig()

            def epi_a(hl, G):
                gi = hl * 4 + G
                bo = 4 + gi % 2
                ON = ON2[gi % 2]
                DVE.wait(PE.sig(), st.get("on_free%d" % (gi % 2)))
                ov = PSB[bo][:, 0:260].rearrange("p (q c) -> p q c", q=4)
                DVE(nc.vector.reciprocal(out=RI.unsqueeze(2), in_=ov[:, :, 64:65]))
                DVE.fence()
                for qq in range(4):
                    DVE(nc.vector.tensor_scalar(out=ON[:, qq, :], in0=ov[:, qq, 0:64], scalar1=RI[:, qq:qq + 1], scalar2=None, op0=ALU.mult))
                bank_free[bo] = DVE.sig()
                return bank_free[bo]

            def epi_b(hl, G, atok):
                gi = hl * 4 + G
                hp = slice(hl * 64, (hl + 1) * 64)
                ON = ON2[gi % 2]
                SQn = SQn2[gi % 2]
                ACT.wait(atok)
                for qq in range(4):
                    ACT(nc.scalar.activation(out=SQn[:, qq, :], in_=ON[:, qq, :], func=AF.Square, accum_out=S4b[:, qq:qq + 1]))
                ACT(nc.scalar.activation(out=S4c, in_=S4b, func=AF.Sqrt, bias=epsb[:, 0:1], scale=1.0 / 64))
                DVE.wait(ACT.sig())
                DVE(nc.vector.reciprocal(out=S4, in_=S4c))
                DVE.fence()
                for qq in range(4):
                    DVE(nc.vector.tensor_scalar(out=ATTt[:, G * 4 + qq, hp], in0=ON[:, qq, :], scalar1=S4[:, qq:qq + 1], scalar2=None, op0=ALU.mult))
                st["on_free%d" % (gi % 2)] = DVE.sig()

            pending = []
            nst = len(steps)
            for i in range(nst + 1):
                if i < nst:
                    emit_scores(i)
                if i >= 1:
                    emit_pv(i - 1)
                    hl_, G_, kt_ = steps[i - 1]
                    if kt_ == 4 * G_ + 3:
                        atok = epi_a(hl_, G_)
                        pending.append((i + 2, hl_, G_, atok))
                while pending and (pending[0][0] <= i or i == nst):
                    _, hl_, G_, atok = pending.pop(0)
                    epi_b(hl_, G_, atok)
            if p == 0:
                dump(ar(U0, 2048), 11264, "b")
            PE.wait(DVE.sig())
            for half in range(2):
                pe_bank(7)
                for c in range(8):
                    t = half * 8 + c
                    PE(nc.tensor.transpose(out=B7[:, c * 128:(c + 1) * 128], in_=ATTt[:, t, :], identity=cs["ident_bf"][:]))
                ptok = PE.sig()
                ACT.wait(ptok)
                ACT(nc.scalar.activation(out=mixT[:, 4 + p, half * 1024:(half + 1) * 1024], in_=B7, func=AF.Copy, scale=beta_a[:, p:p + 1]))
                bank_free[7] = ACT.sig()
            return slab_done

        for ui, u in enumerate(units):
            s_, v, tok = pend
            barrier()
            if ui + 1 < len(units):
                with nc.allow_non_contiguous_dma(reason="256B weight rows"):
                    pend = load_slab(units[ui + 1])
            if u < 4:
                done = ret_unit(u, v, tok)
            else:
                done = att_unit(u - 4, v, tok)
            slr.free[s_] = done
        barrier()
        if stop_after == "mixcat":
            for c in range(8):
                DVE(nc.vector.tensor_copy(out=XT[:, c, :], in_=mixT[:, c, :]))
            return
        SP.wait(wtok["w_out"])
        wo_tok = wor.slots[0].start(SP, WO, wb["w_out"].rearrange("(kc p) c -> p kc c", p=128))
        PE.wait(wo_tok)
        oi = 0
        for dc in range(8):
            for g in range(4):
                blk = slice(g * 512, (g + 1) * 512)
                b = 4 + oi % 2
                oi += 1
                pe_bank(b)
                for c in range(8):
                    PE(nc.tensor.matmul(PSB[b][:], lhsT=WO[:, c, dc * 128:(dc + 1) * 128], rhs=mixT[:, c, blk], start=(c == 0), stop=(c == 7)))
                DVE.wait(PE.sig())
                DVE(nc.vector.tensor_tensor(out=XT[:, dc, blk], in0=PSB[b][:], in1=XT[:, dc, blk], op=ALU.add))
                bank_free[b] = DVE.sig()

    barrier()
    for s in range(NSEQ):
        if s > 0:
            for E in ENGS:
                E.new_epoch()
        stage_load(s)
        barrier()
        if stop_after != "load":
            stage_ffn("ffn1")
            barrier()
        if stop_after not in ("load", "ffn1"):
            stage_mix()
            barrier()
            if stop_after not in ("mix", "mixcat"):
                stage_ffn("ffn2")
                barrier()
        stage_out(s)
        barrier()
    SP.wait(*[f for f in oring.free if f is not None])
    return nc


def kernel(**inputs):
    NCORES = 8
    NSEQ = 4
    nc = build(NSEQ)
    C = _consts()
    xs = np.ascontiguousarray(inputs["x"]).reshape(NCORES, NSEQ, S, D)
    base = {}
    for name, shp in WSPECS:
        base[name] = np.ascontiguousarray(np.asarray(inputs[name], dtype=np.float32).reshape(shp))
    for name, n in VSPECS:
        base[name] = np.ascontiguousarray(np.asarray(inputs[name], dtype=np.float32).reshape(n))
    for name, shp, dt in CONST_SPECS:
        base["c_" + name] = np.ascontiguousarray(C[name])
    in_maps = []
    for c in range(NCORES):
        m = dict(base)
        m["x"] = xs[c]
        in_maps.append(m)
    res = run_bass_kernel_spmd(nc, in_maps, core_ids=list(range(NCORES)))
    out = np.stack([r["y"] for r in res.results], axis=0).reshape(NCORES * NSEQ, S, D)
    return out.astype(np.float32)
```

```python
import os
import numpy as np
import ml_dtypes
import concourse.bass as bass
import concourse.mybir as mybir
from concourse.bass_utils import run_bass_kernel_spmd

F32 = mybir.dt.float32
BF16 = mybir.dt.bfloat16
AF = mybir.ActivationFunctionType
ALU = mybir.AluOpType
AX = mybir.AxisListType

S = 2048
D = 1024
FF = 2816
NJ = 22
NT = 16
EPS = 1e-6
NEG = -30000.0
FENCE_ALL = bool(os.environ.get("K_FENCE_ALL"))


class Eng:
    def __init__(self, nc, e, name):
        self.nc, self.e, self.name = nc, e, name
        self.ep = 0
        self.sem = nc.alloc_semaphore(f"{name}_e0")
        self.cnt = 0
        self.waited = {}
        self.last_ins = None
        self.last_tok = None
        self.mysems = {self.sem.name}

    def new_epoch(self):
        self.ep += 1
        self.sem = self.nc.alloc_semaphore(f"{self.name}_e{self.ep}")
        self.mysems.add(self.sem.name)
        self.cnt = 0
        self.last_ins = None
        self.last_tok = None

    def __call__(self, ins):
        self.last_ins = ins
        self.last_tok = None
        if FENCE_ALL:
            sem, v = self.sig()
            self.e.wait_ge(sem, v)
        return ins

    def sig(self):
        if self.last_tok is None:
            assert self.last_ins is not None
            self.cnt += 1
            self.last_ins.then_inc(self.sem, 1)
            self.last_tok = (self.sem, self.cnt)
        return self.last_tok

    def fence(self):
        sem, v = self.sig()
        self.e.wait_ge(sem, v)

    def wait(self, *toks):
        for tok in toks:
            if tok is None:
                continue
            if isinstance(tok, (list, tuple)) and len(tok) and isinstance(tok[0], (list, tuple)):
                self.wait(*tok)
                continue
            sem, v = tok
            if sem.name in self.mysems:
                continue
            if self.waited.get(sem.name, 0) >= v:
                continue
            self.e.wait_ge(sem, v)
            self.waited[sem.name] = v


class DmaSlot:
    def __init__(self, nc, name):
        self.sem = nc.alloc_semaphore(name)
        self.cnt = 0

    def start(self, q, out, in_):
        q.e.dma_start(out=out, in_=in_).then_inc(self.sem, 16)
        self.cnt += 16
        return (self.sem, self.cnt)


class Queue:
    def __init__(self, e):
        self.e = e
        self.waited = {}

    def wait(self, *toks):
        for tok in toks:
            if tok is None:
                continue
            if isinstance(tok, (list, tuple)) and len(tok) and isinstance(tok[0], (list, tuple)):
                self.wait(*tok)
                continue
            sem, v = tok
            if self.waited.get(sem.name, 0) >= v:
                continue
            self.e.wait_ge(sem, v)
            self.waited[sem.name] = v


def _consts():
    c = {}
    bf = ml_dtypes.bfloat16
    c["ident_bf"] = np.eye(128, dtype=np.float32).astype(bf)
    c["ident_f"] = np.eye(128, dtype=np.float32)
    c["ones_bf"] = np.ones((128, 128), np.float32).astype(bf)
    pm = np.zeros((128, 128), np.float32)
    for dp in range(64):
        pm[dp + 64, dp] = -1.0
        pm[dp, dp + 64] = 1.0
    c["pm_bf"] = pm.astype(bf)
    pos = np.arange(S, dtype=np.float32)
    invf = (10000.0 ** (-np.arange(64, dtype=np.float32) * 2.0 / 128.0)).astype(np.float32)
    ang = pos[None, :] * invf[np.arange(128) % 64][:, None]
    c["cos_r"] = np.cos(ang).astype(np.float32).astype(bf)
    c["sin_r"] = np.sin(ang).astype(np.float32).astype(bf)
    g = 1.0 - 2.0 ** (-5.0 - np.arange(4, dtype=np.float64))
    i = np.arange(128, dtype=np.float64)
    qd = g[:, None] ** i[None, :]
    kd = (g[:, None] ** (-i[None, :])) * (128.0 ** -0.5)
    c["qdec"] = np.broadcast_to(qd[None].astype(np.float32), (128, 4, 128)).copy()
    c["kdec"] = np.broadcast_to(kd[None].astype(np.float32), (128, 4, 128)).copy()
    c["gC"] = [float(x) for x in (g ** 128)]
    jj = np.arange(128)
    c["maskT"] = (jj[:, None] <= jj[None, :]).astype(np.float32)
    c["cbT"] = np.where(jj[:, None] <= jj[None, :], 0.0, NEG).astype(np.float32).astype(bf)
    invfa = (500000.0 ** (-np.arange(8, dtype=np.float32) * 2.0 / 16.0)).astype(np.float32)
    p = np.arange(128)
    t = np.arange(NT)
    posa = (t[None, :] * 128 + p[:, None]).astype(np.float32)
    anga = posa[:, :, None] * invfa[None, None, :]
    c["cos_a"] = np.cos(anga).astype(np.float32)
    c["sin_a"] = np.sin(anga).astype(np.float32)
    e32 = np.zeros((64, 8, 128), np.float32)
    for n in range(8):
        e32[n, n, :] = 1.0
        e32[32 + n, n, :] = 1.0
    c["e32"] = e32.astype(bf)
    pmk = np.zeros((128, NT, 8), np.float32)
    for tt in range(NT):
        own = tt // 2
        pmk[:, tt, own:] = -1e30
    c["pastmask"] = pmk
    return c


CONST_SPECS = [
    ("ident_bf", [128, 128], BF16), ("ident_f", [128, 128], F32), ("ones_bf", [128, 128], BF16),
    ("pm_bf", [128, 128], BF16), ("cos_r", [128, S], BF16), ("sin_r", [128, S], BF16),
    ("qdec", [128, 4, 128], F32), ("kdec", [128, 4, 128], F32), ("maskT", [128, 128], F32),
    ("cbT", [128, 128], BF16), ("cos_a", [128, NT, 8], F32), ("sin_a", [128, NT, 8], F32),
    ("e32", [64, 8, 128], BF16), ("pastmask", [128, NT, 8], F32),
]

WSPECS = [
    ("ffn1_w_gate", [D, FF]), ("ffn1_w_up", [D, FF]), ("ffn1_w_down", [FF, D]),
    ("w_in", [D, 3584]), ("w_out", [D, D]),
    ("ffn2_w_gate", [D, FF]), ("ffn2_w_up", [D, FF]), ("ffn2_w_down", [FF, D]),
]
VSPECS = [("ffn1_norm_w", D), ("mix_norm_w", D), ("ffn2_norm_w", D), ("ret_out_beta", 512),
          ("att_out_beta", 512), ("q_norm_w", 64), ("k_norm_w", 64)]


def build(NSEQ=4, stop_after="all", do_cast=True, MIX_UNITS=None, DEBUG=False):
    C = _consts()
    nc = bass.Bass("TRN2", target_bir_lowering=False)
    x = nc.dram_tensor("x", [NSEQ, S, D], F32, kind="ExternalInput").ap()
    y = nc.dram_tensor("y", [NSEQ, S, D], F32, kind="ExternalOutput").ap()
    wf = {}
    wb = {}
    for name, shp in WSPECS:
        wf[name] = nc.dram_tensor(name, shp, F32, kind="ExternalInput").ap()
        wb[name] = nc.dram_tensor(name + "_bf", shp, BF16, kind="Internal").ap()
    vin = {}
    for name, n in VSPECS:
        vin[name] = nc.dram_tensor(name, [n], F32, kind="ExternalInput").ap()
    cd = {}
    for name, shp, dt in CONST_SPECS:
        cd[name] = nc.dram_tensor("c_" + name, shp, dt, kind="ExternalInput").ap()

    def sb(name, shp, dt):
        return nc.alloc_sbuf_tensor(name, shp, dt)

    dbg = {}
    if DEBUG:
        dbg["f"] = nc.dram_tensor("dbgf", [128, 16384], F32, kind="ExternalOutput").ap()
        dbg["b"] = nc.dram_tensor("dbgb", [128, 32768], BF16, kind="ExternalOutput").ap()
        dbg["sem"] = None
        dbg["toks"] = []

    def dump(ap2d, off, kind):
        if not DEBUG:
            return
        barrier()
        if dbg["sem"] is None:
            dbg["sem"] = DmaSlot(nc, "dbgsem")
        n = ap2d.shape[1]
        np_ = ap2d.shape[0]
        tok = dbg["sem"].start(SP, dbg[kind][0:np_, off:off + n], ap2d)
        SP.wait(tok)
        for E in ENGS:
            E.wait(tok)

    XT = sb("XT", [128, 8, S], F32)
    ARENA_N = 58432
    ARENA = sb("ARENA", [128, ARENA_N], BF16)
    cs = {}
    for name, shp, dt in CONST_SPECS:
        cs[name] = sb("k_" + name, shp, dt)
    nw = {k: sb("nw_" + k, [128, 8], F32) for k in ("ffn1_norm_w", "mix_norm_w", "ffn2_norm_w")}
    beta_r = sb("beta_r", [128, 4], F32)
    beta_a = sb("beta_a", [128, 4], F32)
    wqk = sb("wqk", [128, 4, 64], F32)
    RT = sb("RT", [128, 512], F32)
    RS = RT
    SG = [sb(f"SG{i}", [128, 512], BF16) for i in range(2)]
    QD = sb("QD", [128, 512], BF16)
    U32 = [sb(f"U32_{h}", [128, 128], F32) for h in range(1)]
    SBF = sb("SBF", [128, 128], BF16)
    SBFb = sb("SBFb", [128, 128], BF16)
    SCM = [sb(f"SCM{i}", [128, 128], BF16) for i in range(2)]
    ST = sb("ST", [128, 256], F32)
    GM = sb("GM", [128, NT, 2, 8], F32)
    TOP = sb("TOP", [128, NT, 2, 8], F32)
    ZB = sb("ZB", [128, 320], BF16)

    PSB = [nc.alloc_psum_tensor(f"PS{i}", [128, 512], F32) for i in range(8)]
    bank_free = [None] * 8

    PE = Eng(nc, nc.tensor, "pe")
    ACT = Eng(nc, nc.scalar, "act")
    DVE = Eng(nc, nc.vector, "dve")
    POOL = Eng(nc, nc.gpsimd, "pool")
    SP = Queue(nc.sync)
    ENGS = [PE, ACT, DVE, POOL]

    def ar(off, n):
        return ARENA[:, off:off + n]

    dsl = DmaSlot(nc, "cst")
    ctoks = []
    for name, shp, dt in CONST_SPECS:
        ctoks.append(dsl.start(SP, cs[name][:], cd[name]))
    with nc.allow_non_contiguous_dma(reason="tiny param layouts"):
        for k in nw:
            ctoks.append(dsl.start(SP, nw[k][:], vin[k].rearrange("(k p) -> p k", p=128)))
        ctoks.append(dsl.start(SP, beta_r[:], vin["ret_out_beta"].rearrange("(k p) -> p k", p=128)))
        ctoks.append(dsl.start(SP, beta_a[:], vin["att_out_beta"].rearrange("(k p) -> p k", p=128)))
        for g4, nm in enumerate(["q_norm_w", "q_norm_w", "k_norm_w", "k_norm_w"]):
            ctoks.append(dsl.start(SP, wqk[:, g4, :], vin[nm].partition_broadcast(128)))
    ctok = ctoks[-1]
    wtok = {}
    CH = 4096
    FST = [ARENA[:, i * 8192:(i + 1) * 8192].bitcast(F32) for i in range(2)]
    BST = [ARENA[:, 16384 + i * 4096:16384 + (i + 1) * 4096] for i in range(2)]
    ldr = [DmaSlot(nc, f"pl{i}") for i in range(2)]
    str_ = [DmaSlot(nc, f"ps{i}") for i in range(2)]
    f_free = [None, None]
    st_tok = [None, None]
    ci = 0
    cast_engs = None
    for name, shp in WSPECS:
        if not do_cast:
            wtok[name] = None
            continue
        R, Cc = shp
        n = R * Cc // 128
        src = wf[name].rearrange("r c -> (r c)").rearrange("(p n) -> p n", p=128)
        dst = wb[name].rearrange("r c -> (r c)").rearrange("(p n) -> p n", p=128)
        last = None
        for c0 in range(0, n, CH):
            c1 = min(n, c0 + CH)
            w_ = c1 - c0
            sl = ci % 2
            SP.wait(f_free[sl])
            ltok = ldr[sl].start(SP, FST[sl][:, 0:w_], src[:, c0:c1])
            E = [ACT, DVE][ci % 2]
            E.wait(ltok, st_tok[sl])
            if E is ACT:
                ACT(nc.scalar.copy(out=BST[sl][:, 0:w_], in_=FST[sl][:, 0:w_]))
            elif E is DVE:
                DVE(nc.vector.tensor_copy(out=BST[sl][:, 0:w_], in_=FST[sl][:, 0:w_]))
            else:
                POOL(nc.gpsimd.tensor_copy(out=BST[sl][:, 0:w_], in_=FST[sl][:, 0:w_]))
            ctk = E.sig()
            f_free[sl] = ctk
            SP.wait(ctk)
            st_tok[sl] = str_[sl].start(SP, dst[:, c0:c1], BST[sl][:, 0:w_])
            last = st_tok[sl]
            ci += 1
        wtok[name] = [t for t in st_tok if t is not None]
    for E in ENGS:
        E.wait(ctok)
    DVE(nc.vector.memset(ZB[:], 0.0))
    zb_tok = DVE.sig()
    PE.wait(zb_tok)
    SP.wait(*[t for t in st_tok if t is not None])

    def barrier():
        toks = [E.sig() for E in ENGS if E.last_ins is not None]
        for E in ENGS:
            E.wait(*toks)
        SP.wait(*toks)

    def pe_bank(b):
        PE.wait(bank_free[b])

    class Ring:
        def __init__(self, name, nslots):
            self.slots = [DmaSlot(nc, f"{name}{i}") for i in range(nslots)]
            self.free = [None] * nslots
            self.n = nslots
            self.i = 0

        def load(self, pairs, wait_tok=None):
            s = self.i % self.n
            self.i += 1
            SP.wait(self.free[s], wait_tok)
            tok = None
            for o, i_ in pairs:
                tok = self.slots[s].start(SP, o, i_)
            return s, tok

    xring = Ring("xr", 2)
    oring = Ring("or", 2)
    wgr = Ring("wg", 2)
    wdr = Ring("wd", 2)
    slr = Ring("sl", 2)
    wor = Ring("wo", 1)

    XST = [ARENA[:, 8192 + i * 2048: 8192 + (i + 1) * 2048].bitcast(F32) for i in range(2)]

    def stage_load(s):
        for t in range(NT):
            sl = xring.i % 2
            xring.i += 1
            SP.wait(xring.free[sl], oring.free[sl])
            tok = xring.slots[sl].start(SP, XST[sl], x[s, t * 128:(t + 1) * 128, :])
            PE.wait(tok)
            for hb in range(2):
                b = hb
                pe_bank(b)
                for k in range(4):
                    dc = hb * 4 + k
                    PE(nc.tensor.transpose(out=PSB[b][:, k * 128:(k + 1) * 128], in_=XST[sl][:, dc * 128:(dc + 1) * 128],
                                           identity=cs["ident_f"][:]))
                ptok = PE.sig()
                E = ACT if hb == 0 else DVE
                E.wait(ptok)
                src = PSB[b][:].rearrange("p (k c) -> p k c", k=4)
                dst = XT[:, hb * 4:(hb + 1) * 4, t * 128:(t + 1) * 128]
                if E is ACT:
                    ACT(nc.scalar.copy(out=dst, in_=src))
                else:
                    DVE(nc.vector.tensor_copy(out=dst, in_=src))
                bank_free[b] = E.sig()
            xring.free[sl] = ptok

    def stage_out(s):
        for t in range(NT):
            sl = oring.i % 2
            oring.i += 1
            for hb in range(2):
                b = hb
                pe_bank(b)
                for k in range(4):
                    dc = hb * 4 + k
                    PE(nc.tensor.transpose(out=PSB[b][:, k * 128:(k + 1) * 128], in_=XT[:, dc, t * 128:(t + 1) * 128],
                                           identity=cs["ident_f"][:]))
                ptok = PE.sig()
                E = ACT if hb == 0 else DVE
                E.wait(ptok, oring.free[sl])
                dst = XST[sl][:, hb * 512:(hb + 1) * 512]
                if E is ACT:
                    ACT(nc.scalar.copy(out=dst, in_=PSB[b][:]))
                else:
                    DVE(nc.vector.tensor_copy(out=dst, in_=PSB[b][:]))
                bank_free[b] = E.sig()
            SP.wait(bank_free[0], bank_free[1])
            oring.free[sl] = oring.slots[sl].start(SP, y[s, t * 128:(t + 1) * 128, :], XST[sl])

    def stage_norm(hT, t0, ntok, wname, SQoff):
        SQ = ARENA[:, SQoff:SQoff + 4096].rearrange("p (k c) -> p k c", k=8)
        w = nw[wname]
        for b0 in range(0, ntok, 512):
            ACT.wait(norm_state["sq_free"])
            for kc in range(8):
                ACT(nc.scalar.activation(out=SQ[:, kc, :], in_=XT[:, kc, t0 + b0:t0 + b0 + 512], func=AF.Square))
            sqtok = ACT.sig()
            PE.wait(sqtok)
            pe_bank(5)
            for kc in range(8):
                PE(nc.tensor.matmul(PSB[5][:], lhsT=cs["ones_bf"][:], rhs=SQ[:, kc, :], start=(kc == 0), stop=(kc == 7)))
            ptok = PE.sig()
            norm_state["sq_free"] = ptok
            ACT.wait(ptok, norm_state["rt_free"])
            ACT(nc.scalar.activation(out=RT[:], in_=PSB[5][:], func=AF.Sqrt, bias=epsb[:, 0:1], scale=1.0 / D))
            rttok = ACT.sig()
            bank_free[5] = rttok
            DVE.wait(rttok)
            DVE(nc.vector.reciprocal(out=RS[:], in_=RT[:]))
            for kc in range(8):
                DVE(nc.vector.scalar_tensor_tensor(out=hT[:, kc, b0:b0 + 512], in0=XT[:, kc, t0 + b0:t0 + b0 + 512],
                                                   scalar=w[:, kc:kc + 1], in1=RS[:], op0=ALU.mult, op1=ALU.mult))
            norm_state["rt_free"] = DVE.sig()
        return DVE.sig()

    norm_state = {"sq_free": None, "rt_free": None}
    epsb = sb("epsb", [128, 1], F32)
    DVE(nc.vector.memset(epsb[:], EPS))
    ACT.wait(DVE.sig())

    def stage_ffn(which):
        wg, wu, wd = wb[f"{which}_w_gate"], wb[f"{which}_w_up"], wb[f"{which}_w_down"]
        wgv = wg.rearrange("(kc p) f -> p kc f", p=128)
        wuv = wu.rearrange("(kc p) f -> p kc f", p=128)
        wdv = wd.rearrange("(j p) d -> p j d", p=128)
        wt = [wtok[f"{which}_w_gate"], wtok[f"{which}_w_up"], wtok[f"{which}_w_down"]]
        hT = ar(0, 8192).rearrange("p (k c) -> p k c", k=8)
        actT = ar(8192, 22528).rearrange("p (j c) -> p j c", j=NJ)
        WG = [ar(30720 + i * 2048, 2048).rearrange("p (k c) -> p k c", k=8) for i in range(2)]
        WU = [ar(34816 + i * 2048, 2048).rearrange("p (k c) -> p k c", k=8) for i in range(2)]
        WD = [ar(38912 + i * 5632, 5632).rearrange("p (j c) -> p j c", j=NJ) for i in range(2)]
        NSL = 11
        for G in range(2):
            t0 = G * 1024

            def load_gu(i):
                c0 = i * 256
                s_ = wgr.i % 2
                wgr.i += 1
                SP.wait(wgr.free[s_], wt[0], wt[1])
                wgr.slots[s_].start(SP, WG[s_][:], wgv[:, :, c0:c0 + 256])
                tok = wgr.slots[s_].start(SP, WU[s_][:], wuv[:, :, c0:c0 + 256])
                return s_, tok

            def load_d(i):
                c0 = i * 256
                s_ = wdr.i % 2
                wdr.i += 1
                SP.wait(wdr.free[s_], wt[2])
                tok = wdr.slots[s_].start(SP, WD[s_][:], wdv[:, :, c0:c0 + 256])
                return s_, tok

            pend = load_gu(0)
            pendd = load_d(0)
            htok = stage_norm(hT, t0, 1024, f"{which}_norm_w", 50176)
            PE.wait(htok)
            gi = 0
            for i in range(NSL):
                cur = pend
                if i + 1 < NSL:
                    pend = load_gu(i + 1)
                s_, tok = cur
                PE.wait(tok)
                for jj in range(2):
                    j = i * 2 + jj
                    for th in range(2):
                        bg = (gi % 2) * 2
                        bu = bg + 1
                        gi += 1
                        pe_bank(bg)
                        for kc in range(8):
                            PE(nc.tensor.matmul(PSB[bg][:], lhsT=WG[s_][:, kc, jj * 128:(jj + 1) * 128],
                                                rhs=hT[:, kc, th * 512:(th + 1) * 512], start=(kc == 0), stop=(kc == 7)))
                        gtok = PE.sig()
                        pe_bank(bu)
                        for kc in range(8):
                            PE(nc.tensor.matmul(PSB[bu][:], lhsT=WU[s_][:, kc, jj * 128:(jj + 1) * 128],
                                                rhs=hT[:, kc, th * 512:(th + 1) * 512], start=(kc == 0), stop=(kc == 7)))
                        utok = PE.sig()
                        sg = SG[(gi) % 2]
                        ACT.wait(gtok, ffn_state["sg_free"][gi % 2])
                        ACT(nc.scalar.activation(out=sg[:], in_=PSB[bg][:], func=AF.Silu))
                        stok = ACT.sig()
                        bank_free[bg] = stok
                        DVE.wait(stok, utok, ffn_state["act_free"])
                        DVE(nc.vector.tensor_tensor(out=actT[:, j, th * 512:(th + 1) * 512], in0=PSB[bu][:], in1=sg[:], op=ALU.mult))
                        dtok = DVE.sig()
                        bank_free[bu] = dtok
                        ffn_state["sg_free"][gi % 2] = dtok
                wgr.free[s_] = PE.sig()
            hT_free = PE.sig()
            acttok = DVE.sig()
            PE.wait(acttok)
            oi = 0
            for i in range(4):
                cur = pendd
                if i + 1 < 4:
                    pendd = load_d(i + 1)
                s_, tok = cur
                PE.wait(tok)
                for dl in range(2):
                    dc = i * 2 + dl
                    for th in range(2):
                        b = 4 + (oi % 2)
                        oi += 1
                        pe_bank(b)
                        for j in range(NJ):
                            PE(nc.tensor.matmul(PSB[b][:], lhsT=WD[s_][:, j, dl * 128:(dl + 1) * 128],
                                                rhs=actT[:, j, th * 512:(th + 1) * 512], start=(j == 0), stop=(j == NJ - 1)))
                        otok = PE.sig()
                        DVE.wait(otok)
                        xs = XT[:, dc, t0 + th * 512:t0 + (th + 1) * 512]
                        DVE(nc.vector.scalar_tensor_tensor(out=xs, in0=PSB[b][:], scalar=0.5, in1=xs, op0=ALU.mult, op1=ALU.add))
                        bank_free[b] = DVE.sig()
                wdr.free[s_] = PE.sig()
            ffn_state["act_free"] = PE.sig()
            DVE.wait(hT_free)
            ACT.wait(DVE.sig())

    ffn_state = {"sg_free": [None, None], "act_free": None}

    def stage_mix():
        hT2 = ar(0, 16384).rearrange("p (k c) -> p k c", k=8)
        mixT = ar(16384, 16384).rearrange("p (k c) -> p k c", k=8)
        SLAB = [ar(32768 + i * 4096, 4096) for i in range(2)]
        U0 = 40960
        qT = ar(U0, 2048)
        kT = ar(U0 + 2048, 2048)
        vtok = ar(U0 + 4096, 2048).rearrange("p (t c) -> p t c", t=NT)
        gs = ar(U0 + 6144, 2048)
        ktok = ar(U0 + 8192, 2048).rearrange("p (t c) -> p t c", t=NT)
        QKt = ar(U0, 4096).rearrange("p (t g c) -> p t g c", t=NT, g=4)
        ATTt = ar(U0, 2048).rearrange("p (t c) -> p t c", t=NT)
        PT = [ar(U0 + 2048 + i * 512, 512) for i in range(4)]
        vext = ar(U0 + 4096, 2080).rearrange("p (t h c) -> p t h c", t=NT, h=2)
        qTp = ar(U0 + 6208, 2048)
        kTp = ar(U0 + 8256, 2048)
        biasT = ar(U0 + 10304, 2048)
        BIASt = ar(U0 + 12352, 1024).rearrange("p (t h c) -> p t h c", t=NT, h=2)
        WO = ar(U0, 8192).rearrange("p (k c) -> p k c", k=8)
        SQo = 54336
        TA = ar(SQo, 2048).bitcast(F32)
        T1 = TA[:, 0:512]
        T2 = TA[:, 512:1024]
        SQ1 = ar(SQo + 2048, 512)
        TB = ar(SQo + 2560, 1024).bitcast(F32)
        winv = wb["w_in"].rearrange("(kc p) c -> p kc c", p=128)
        B7 = PSB[7][:].bitcast(BF16)
        B6 = PSB[6][:].bitcast(BF16)
        gCs = C["gC"]

        def load_slab(u):
            s_ = slr.i % 2
            slr.i += 1
            SP.wait(slr.free[s_], wtok["w_in"])
            tok = None
            if u < 4:
                v = SLAB[s_].rearrange("p (k a c) -> p k a c", k=8, a=4)
                for part in range(4):
                    c0 = part * 512 + u * 128
                    tok = slr.slots[s_].start(SP, v[:, :, part, :], winv[:, :, c0:c0 + 128])
            else:
                v = SLAB[s_][:, 0:3072].rearrange("p (k a c) -> p k a c", k=8, a=3)
                for part in range(3):
                    c0 = 2048 + part * 512 + (u - 4) * 128
                    tok = slr.slots[s_].start(SP, v[:, :, part, :], winv[:, :, c0:c0 + 128])
            return s_, v, tok

        units = list(range(8)) if MIX_UNITS is None else MIX_UNITS
        with nc.allow_non_contiguous_dma(reason="256B weight rows"):
            pend = load_slab(units[0])
        htok = stage_norm(hT2, 0, S, "mix_norm_w", SQo)
        PE.wait(htok)
        ACT.wait(htok)
        st = {"scm_free": [None, None], "pt_free": [None] * 4, "sbf_tok": None, "sbf_free": None}

        def ret_unit(h, slab, stok):
            PE.wait(stok)
            bi = 0
            for g in range(4):
                blk = slice(g * 512, (g + 1) * 512)
                for part, dec, dstT in ((0, cs["qdec"], qT), (1, cs["kdec"], kT)):
                    bq = bi % 2
                    br = 2 + bi % 2
                    bi += 1
                    pe_bank(bq)
                    for kc in range(8):
                        PE(nc.tensor.matmul(PSB[bq][:], lhsT=slab[:, kc, part, :], rhs=hT2[:, kc, blk], start=(kc == 0), stop=(kc == 7)))
                    ptok = PE.sig()
                    DVE.wait(ptok)
                    DVE(nc.vector.tensor_tensor(out=QD[:].rearrange("p (a c) -> p a c", a=4),
                                                in0=PSB[bq][:].rearrange("p (a c) -> p a c", a=4),
                                                in1=dec[:, h, :].unsqueeze(1).broadcast_to([128, 4, 128]), op=ALU.mult))
                    qdtok = DVE.sig()
                    bank_free[bq] = qdtok
                    PE.wait(qdtok)
                    pe_bank(br)
                    PE(nc.tensor.matmul(PSB[br][:], lhsT=cs["pm_bf"][:], rhs=QD[:], start=True, stop=True))
                    rtok = PE.sig()
                    DVE(nc.vector.tensor_tensor(out=T1, in0=QD[:], in1=cs["cos_r"][:, blk], op=ALU.mult))
                    DVE.wait(rtok)
                    DVE(nc.vector.tensor_tensor(out=T2, in0=PSB[br][:], in1=cs["sin_r"][:, blk], op=ALU.mult))
                    bank_free[br] = DVE.sig()
                    DVE(nc.vector.tensor_tensor(out=dstT[:, blk], in0=T1, in1=T2, op=ALU.add))
                bq = bi % 2
                bi += 1
                pe_bank(bq)
                for kc in range(8):
                    PE(nc.tensor.matmul(PSB[bq][:], lhsT=slab[:, kc, 3, :], rhs=hT2[:, kc, blk], start=(kc == 0), stop=(kc == 7)))
                ptok = PE.sig()
                ACT.wait(ptok)
                ACT(nc.scalar.activation(out=gs[:, blk], in_=PSB[bq][:], func=AF.Silu))
                bank_free[bq] = ACT.sig()
                bq = bi % 2
                bi += 1
                pe_bank(bq)
                for tl in range(4):
                    t = g * 4 + tl
                    for kc in range(8):
                        PE(nc.tensor.matmul(PSB[bq][:, tl * 128:(tl + 1) * 128], lhsT=hT2[:, kc, t * 128:(t + 1) * 128],
                                            rhs=slab[:, kc, 2, :], start=(kc == 0), stop=(kc == 7)))
                ptok = PE.sig()
                ACT.wait(ptok)
                ACT(nc.scalar.copy(out=vtok[:, g * 4:(g + 1) * 4, :], in_=PSB[bq][:].rearrange("p (t c) -> p t c", t=4)))
                bank_free[bq] = ACT.sig()
            slab_done = PE.sig()
            qk_tok_ = DVE.sig()
            v_tok_ = ACT.sig()
            PE.wait(qk_tok_, v_tok_)
            for half in range(2):
                pe_bank(7)
                for c in range(8):
                    n = half * 8 + c
                    PE(nc.tensor.transpose(out=B7[:, c * 128:(c + 1) * 128], in_=kT[:, n * 128:(n + 1) * 128], identity=cs["ident_bf"][:]))
                ptok = PE.sig()
                ACT.wait(ptok)
                ACT(nc.scalar.copy(out=ktok[:, half * 8:(half + 1) * 8, :], in_=B7.rearrange("p (t c) -> p t c", t=8)))
                bank_free[7] = ACT.sig()
            PE.wait(ACT.sig())
            U = U32[0]
            SBF2 = [SBF, SBFb]
            toks = {}

            def o_part(n):
                ch = slice(n * 128, (n + 1) * 128)
                bo = 4 + (n // 4) % 2
                col = (n % 4) * 128
                PE.wait(toks["scm", n])
                if n % 4 == 0:
                    pe_bank(bo)
                PE(nc.tensor.matmul(PSB[bo][:, col:col + 128], lhsT=vtok[:, n, :], rhs=SCM[n % 2][:], start=True, stop=(n == 0)))
                if n > 0:
                    PE.wait(toks["sbf", n])
                    PE(nc.tensor.matmul(PSB[bo][:, col:col + 128], lhsT=SBF2[n % 2][:], rhs=qT[:, ch], start=False, stop=True))
                otok = PE.sig()
                toks["o", n] = otok
                st["scm_free"][n % 2] = otok

            def epi(n):
                bo = 4 + (n // 4) % 2
                blk = slice((n // 4) * 512, (n // 4 + 1) * 512)
                ACT.wait(toks["o", n])
                ACT(nc.scalar.activation(out=SQ1, in_=PSB[bo][:], func=AF.Square))
                PE.wait(ACT.sig())
                pe_bank(6)
                PE(nc.tensor.matmul(PSB[6][:], lhsT=cs["ones_bf"][:], rhs=SQ1, start=True, stop=True))
                ACT.wait(PE.sig())
                ACT(nc.scalar.activation(out=RT[:], in_=PSB[6][:], func=AF.Sqrt, bias=epsb[:, 0:1], scale=1.0 / 128))
                rt_ = ACT.sig()
                bank_free[6] = rt_
                DVE.wait(rt_)
                DVE(nc.vector.reciprocal(out=RS[:], in_=RT[:]))
                DVE(nc.vector.tensor_tensor(out=T1, in0=PSB[bo][:], in1=RS[:], op=ALU.mult))
                bank_free[bo] = DVE.sig()
                DVE(nc.vector.scalar_tensor_tensor(out=mixT[:, h, blk], in0=T1, scalar=beta_r[:, h:h + 1], in1=gs[:, blk],
                                                   op0=ALU.mult, op1=ALU.mult))
                ACT.wait(DVE.sig())

            for n in range(NT + 1):
                if n < NT:
                    ch = slice(n * 128, (n + 1) * 128)
                    bs = n % 2
                    bk = 2 + n % 2
                    pe_bank(bs)
                    PE(nc.tensor.matmul(PSB[bs][:, 0:128], lhsT=kT[:, ch], rhs=qT[:, ch], start=True, stop=True))
                    sctok = PE.sig()
                    if n < NT - 1:
                        pe_bank(bk)
                        PE(nc.tensor.matmul(PSB[bk][:, 0:128], lhsT=ktok[:, n, :], rhs=vtok[:, n, :], start=True, stop=True))
                        kvtok = PE.sig()
                    DVE.wait(sctok, st["scm_free"][n % 2])
                    DVE(nc.vector.tensor_tensor(out=SCM[n % 2][:], in0=PSB[bs][:, 0:128], in1=cs["maskT"][:], op=ALU.mult))
                    toks["scm", n] = DVE.sig()
                    bank_free[bs] = toks["scm", n]
                if n >= 1:
                    o_part(n - 1)
                if n < NT - 1:
                    DVE.wait(kvtok, toks.get(("sbf", n)))
                    if n == 0:
                        DVE(nc.vector.tensor_copy(out=U[:], in_=PSB[bk][:, 0:128]))
                    else:
                        DVE(nc.vector.scalar_tensor_tensor(out=U[:], in0=U[:], scalar=gCs[h], in1=PSB[bk][:, 0:128], op0=ALU.mult, op1=ALU.add))
                    utok = DVE.sig()
                    bank_free[bk] = utok
                    ACT.wait(utok, toks.get(("o", n - 1)))
                    ACT(nc.scalar.activation(out=SBF2[(n + 1) % 2][:], in_=U[:], func=AF.Copy, scale=gCs[h]))
                    toks["sbf", n + 1] = ACT.sig()
                if n >= 1 and (n - 1) % 4 == 3:
                    epi(n - 1)
            return slab_done

        def att_unit(p, slab, stok):
            PE.wait(stok)
            slab2 = slab.rearrange("p k a c -> p k (a c)")
            DVE(nc.vector.memset(vext[:, :, :, 64:65], 1.0))
            DVE(nc.vector.memset(BIASt[:].rearrange("p t h c -> p (t h c)"), 0.0))
            init_tok = DVE.sig()
            ACT.wait(init_tok)
            for t in range(NT):
                b = t % 2
                pe_bank(b)
                for kc in range(8):
                    PE(nc.tensor.matmul(PSB[b][:, 0:384], lhsT=hT2[:, kc, t * 128:(t + 1) * 128], rhs=slab2[:, kc, :], start=(kc == 0), stop=(kc == 7)))
                ptok = PE.sig()
                ACT.wait(ptok)
                ACT(nc.scalar.copy(out=QKt[:, t, :, :], in_=PSB[b][:, 0:256].rearrange("p (g c) -> p g c", g=4)))
                ACT(nc.scalar.copy(out=vext[:, t, :, 0:64], in_=PSB[b][:, 256:384].rearrange("p (h c) -> p h c", h=2)))
                bank_free[b] = ACT.sig()
            slab_done = PE.sig()
            raw_tok = ACT.sig()
            DVE.wait(raw_tok)
            SS = ST[:, 0:64]
            TAv = TA.rearrange("p (t g c) -> p t g c", t=4, g=4)
            ACT.wait(DVE.sig())
            for t in range(NT):
                for gg in range(4):
                    ix = t * 4 + gg
                    ACT(nc.scalar.activation(out=TA[:, 0:64], in_=QKt[:, t, gg, :], func=AF.Square, accum_out=SS[:, ix:ix + 1]))
            ACT(nc.scalar.activation(out=ST[:, 128:192], in_=SS, func=AF.Sqrt, bias=epsb[:, 0:1], scale=1.0 / 64))
            DVE.wait(ACT.sig())
            DVE(nc.vector.reciprocal(out=SS, in_=ST[:, 128:192]))
            DVE.fence()
            RP = TB.rearrange("p (a t g c) -> p a t g c", a=4, t=4, g=4)
            for q4 in range(4):
                src = QKt[:, q4 * 4:(q4 + 1) * 4, :, :]
                rsb = SS[:, q4 * 16:(q4 + 1) * 16].rearrange("p (t g) -> p t g", t=4).unsqueeze(3).broadcast_to([128, 4, 4, 64])
                for tt in range(4):
                    for gg in range(4):
                        ix = q4 * 16 + tt * 4 + gg
                        DVE(nc.vector.tensor_scalar(out=TAv[:, tt, gg, :], in0=src[:, tt, gg, :], scalar1=SS[:, ix:ix + 1], scalar2=None, op0=ALU.mult))
                DVE(nc.vector.tensor_tensor(out=TAv, in0=TAv, in1=wqk[:].unsqueeze(1).broadcast_to([128, 4, 4, 64]), op=ALU.mult))
                cosb = cs["cos_a"][:, q4 * 4:(q4 + 1) * 4, :].unsqueeze(2).broadcast_to([128, 4, 4, 8])
                sinb = cs["sin_a"][:, q4 * 4:(q4 + 1) * 4, :].unsqueeze(2).broadcast_to([128, 4, 4, 8])
                x1 = TAv[:, :, :, 0:8]
                x2 = TAv[:, :, :, 8:16]
                DVE(nc.vector.tensor_tensor(out=RP[:, 0], in0=x1, in1=cosb, op=ALU.mult))
                DVE(nc.vector.tensor_tensor(out=RP[:, 1], in0=x2, in1=sinb, op=ALU.mult))
                DVE(nc.vector.tensor_tensor(out=RP[:, 2], in0=x2, in1=cosb, op=ALU.mult))
                DVE(nc.vector.tensor_tensor(out=RP[:, 3], in0=x1, in1=sinb, op=ALU.mult))
                DVE(nc.vector.tensor_tensor(out=src[:, :, :, 0:8], in0=RP[:, 0], in1=RP[:, 1], op=ALU.subtract))
                DVE(nc.vector.tensor_tensor(out=src[:, :, :, 8:16], in0=RP[:, 2], in1=RP[:, 3], op=ALU.add))
                DVE(nc.vector.tensor_copy(out=src[:, :, :, 16:64], in_=TAv[:, :, :, 16:64]))
            if p == 0:
                dump(ar(U0, 4096), 0, "b")
                dump(SS, 512, "f")
            PE.wait(DVE.sig())
            for which, dst, gsl in (("k", kTp, slice(2, 4)), ("q", qTp, slice(0, 2))):
                for half in range(2):
                    pe_bank(7)
                    for c in range(8):
                        t = half * 8 + c
                        PE(nc.tensor.transpose(out=B7[:, c * 128:(c + 1) * 128], in_=QKt[:, t, gsl, :].rearrange("p g c -> p (g c)"),
                                               identity=cs["ident_bf"][:]))
                    ptok = PE.sig()
                    ACT.wait(ptok)
                    ACT(nc.scalar.copy(out=dst[:, half * 1024:(half + 1) * 1024], in_=B7))
                    bank_free[7] = ACT.sig()
            qk_dead = PE.sig()
            tr_tok = ACT.sig()
            DVE.wait(tr_tok, qk_dead)
            KS = SCM[0][:, 0:8]
            for nb in range(8):
                ACT(nc.scalar.activation(out=TA[:, 0:256], in_=kTp[:, nb * 256:(nb + 1) * 256], func=AF.Copy, accum_out=ST[:, 16 + nb:17 + nb]))
            ACT(nc.scalar.copy(out=ST[:, 32:40], in_=ST[:, 16:24]))
            DVE.wait(ACT.sig())
            DVE(nc.vector.tensor_copy(out=KS, in_=ST[:, 32:40]))
            PE.wait(DVE.sig(), tr_tok)
            pe_bank(2)
            for t in range(NT):
                for hl in range(2):
                    o_ = (t * 2 + hl) * 8
                    PE(nc.tensor.matmul(PSB[2][:, o_:o_ + 8], lhsT=qTp[hl * 64:(hl + 1) * 64, t * 128:(t + 1) * 128],
                                        rhs=KS[hl * 64:(hl + 1) * 64, :], start=True, stop=True))
            DVE.wait(PE.sig())
            DVE(nc.vector.tensor_tensor(out=GM[:], in0=PSB[2][:, 0:256].rearrange("p (t h n) -> p t h n", t=NT, h=2),
                                        in1=cs["pastmask"][:].unsqueeze(2).broadcast_to([128, NT, 2, 8]), op=ALU.add))
            bank_free[2] = DVE.sig()
            for t in range(NT):
                for hl in range(2):
                    DVE(nc.vector.max(out=TOP[:, t, hl, :], in_=GM[:, t, hl, :]))
            for t in range(NT):
                for hl in range(2):
                    DVE(nc.vector.tensor_tensor(out=GM[:, t, hl, :], in0=GM[:, t, hl, :], in1=TOP[:, t, hl, 2:3].broadcast_to([128, 8]), op=ALU.is_lt))
            DVE(nc.vector.tensor_scalar(out=BIASt[:, :, :, 0:8], in0=GM[:], scalar1=NEG, scalar2=None, op0=ALU.mult))
            PE.wait(DVE.sig())
            for half in range(2):
                pe_bank(7)
                for c in range(8):
                    t = half * 8 + c
                    PE(nc.tensor.transpose(out=B7[0:64, c * 128:(c + 1) * 128], in_=BIASt[:, t, :, :].rearrange("p h c -> p (h c)"),
                                           identity=cs["ident_bf"][:]))
                ptok = PE.sig()
                ACT.wait(ptok)
                ACT(nc.scalar.copy(out=biasT[0:64, half * 1024:(half + 1) * 1024], in_=B7[0:64, :]))
                bank_free[7] = ACT.sig()
            PE.wait(ACT.sig())
            if p == 0:
                dump(qTp, 4096, "b")
                dump(kTp, 6144, "b")
                dump(GM[:].rearrange("p t h n -> p (t h n)"), 0, "f")
                dump(TOP[:].rearrange("p t h n -> p (t h n)"), 256, "f")
                dump(ar(U0 + 12352, 1024), 8192, "b")
                dump(biasT[0:64, :], 9216, "b")
                dump(ar(U0 + 4096, 2080), 13312, "b")
            steps = []
            for hl in range(2):
                for G in range(4):
                    for kt in range(4 * G + 4):
                        steps.append((hl, G, kt))
            ST_ = {}
            RI = ST[:, 0:4]
            S4 = ST[:, 8:12]
            S4b = ST[:, 192:196]
            S4c = ST[:, 200:204]
            ON2 = [TA[:, i * 512:i * 512 + 256].rearrange("p (q c) -> p q c", q=4) for i in range(2)]
            SQn2 = [TA[:, i * 512 + 256:i * 512 + 512].rearrange("p (q c) -> p q c", q=4) for i in range(2)]

            def emit_scores(i):
                hl, G, kt = steps[i]
                hp = slice(hl * 64, (hl + 1) * 64)
                bp = slice(hl * 32, (hl + 1) * 32)
                gi = hl * 4 + G
                bo = 4 + gi % 2
                if kt == 0:
                    pe_bank(bo)
                    PE(nc.tensor.matmul(PSB[bo][:, 0:260], lhsT=ZB[:, 0:128], rhs=ZB[:, 0:260], start=True, stop=False, skip_group_check=True))
                r = kt - 4 * G
                q0 = max(r, 0) * 128
                nblk = kt // 2
                if nblk < 2 * G:
                    b0 = q0
                elif nblk == 2 * G:
                    b0 = 256
                else:
                    b0 = None
                causal = r >= 0
                bs = i % 2
                pe_bank(bs)
                PE(nc.tensor.matmul(PSB[bs][:, q0:512], lhsT=kTp[hp, kt * 128:(kt + 1) * 128], rhs=qTp[hp, G * 512 + q0:(G + 1) * 512],
                                    start=True, stop=(b0 is None and not causal)))
                if b0 is not None:
                    PE(nc.tensor.matmul(PSB[bs][:, b0:512], lhsT=cs["e32"][bp, nblk, :], rhs=biasT[bp, G * 512 + b0:(G + 1) * 512],
                                        start=False, stop=(not causal)))
                if causal:
                    PE(nc.tensor.matmul(PSB[bs][:, r * 128:(r + 1) * 128], lhsT=cs["ident_bf"][:], rhs=cs["cbT"][:], start=False, stop=True))
                sctok = PE.sig()
                slot = i % 4
                ACT.wait(sctok, st["pt_free"][slot])
                ACT(nc.scalar.activation(out=PT[slot][:, q0:512], in_=PSB[bs][:, q0:512], func=AF.Exp, scale=0.125))
                etok = ACT.sig()
                bank_free[bs] = etok
                ST_[i] = etok

            def emit_pv(i):
                hl, G, kt = steps[i]
                gi = hl * 4 + G
                bo = 4 + gi % 2
                r = kt - 4 * G
                slot = i % 4
                PE.wait(ST_[i])
                for qtl in range(max(r, 0), 4):
                    PE(nc.tensor.matmul(PSB[bo][:, qtl * 65:qtl * 65 + 65], lhsT=PT[slot][:, qtl * 128:(qtl + 1) * 128],
                                        rhs=vext[:, kt, hl, :], start=False, stop=(kt == 4 * G + qtl), skip_group_check=True))
                st["pt_free"][slot] = PE.sig()

            def epi_a(hl, G):
                gi = hl * 4 + G
                bo = 4 + gi % 2
                ON = ON2[gi % 2]
                DVE.wait(PE.sig(), st.get("on_free%d" % (gi % 2)))
                ov = PSB[bo][:, 0:260].rearrange("p (q c) -> p q c", q=4)
                DVE(nc.vector.reciprocal(out=RI.unsqueeze(2), in_=ov[:, :, 64:65]))
                DVE.fence()
                for qq in range(4):
                    DVE(nc.vector.tensor_scalar(out=ON[:, qq, :], in0=ov[:, qq, 0:64], scalar1=RI[:, qq:qq + 1], scalar2=None, op0=ALU.mult))
                bank_free[bo] = DVE.sig()
                return bank_free[bo]

            def epi_b(hl, G, atok):
                gi = hl * 4 + G
                hp = slice(hl * 64, (hl + 1) * 64)
                ON = ON2[gi % 2]
                SQn = SQn2[gi % 2]
                ACT.wait(atok)
                for qq in range(4):
                    ACT(nc.scalar.activation(out=SQn[:, qq, :], in_=ON[:, qq, :], func=AF.Square, accum_out=S4b[:, qq:qq + 1]))
                ACT(nc.scalar.activation(out=S4c, in_=S4b, func=AF.Sqrt, bias=epsb[:, 0:1], scale=1.0 / 64))
                DVE.wait(ACT.sig())
                DVE(nc.vector.reciprocal(out=S4, in_=S4c))
                DVE.fence()
                for qq in range(4):
                    DVE(nc.vector.tensor_scalar(out=ATTt[:, G * 4 + qq, hp], in0=ON[:, qq, :], scalar1=S4[:, qq:qq + 1], scalar2=None, op0=ALU.mult))
                st["on_free%d" % (gi % 2)] = DVE.sig()

            pending = []
            nst = len(steps)
            for i in range(nst + 1):
                if i < nst:
                    emit_scores(i)
                if i >= 1:
                    emit_pv(i - 1)
                    hl_, G_, kt_ = steps[i - 1]
                    if kt_ == 4 * G_ + 3:
                        atok = epi_a(hl_, G_)
                        pending.append((i + 2, hl_, G_, atok))
                while pending and (pending[0][0] <= i or i == nst):
                    _, hl_, G_, atok = pending.pop(0)
                    epi_b(hl_, G_, atok)
            if p == 0:
                dump(ar(U0, 2048), 11264, "b")
            PE.wait(DVE.sig())
            for half in range(2):
                pe_bank(7)
                for c in range(8):
                    t = half * 8 + c
                    PE(nc.tensor.transpose(out=B7[:, c * 128:(c + 1) * 128], in_=ATTt[:, t, :], identity=cs["ident_bf"][:]))
                ptok = PE.sig()
                ACT.wait(ptok)
                ACT(nc.scalar.activation(out=mixT[:, 4 + p, half * 1024:(half + 1) * 1024], in_=B7, func=AF.Copy, scale=beta_a[:, p:p + 1]))
                bank_free[7] = ACT.sig()
            return slab_done

        for ui, u in enumerate(units):
            s_, v, tok = pend
            barrier()
            if ui + 1 < len(units):
                with nc.allow_non_contiguous_dma(reason="256B weight rows"):
                    pend = load_slab(units[ui + 1])
            if u < 4:
                done = ret_unit(u, v, tok)
            else:
                done = att_unit(u - 4, v, tok)
            slr.free[s_] = done
        barrier()
        if stop_after == "mixcat":
            for c in range(8):
                DVE(nc.vector.tensor_copy(out=XT[:, c, :], in_=mixT[:, c, :]))
            return
        SP.wait(wtok["w_out"])
        wo_tok = wor.slots[0].start(SP, WO, wb["w_out"].rearrange("(kc p) c -> p kc c", p=128))
        PE.wait(wo_tok)
        oi = 0
        for dc in range(8):
            for g in range(4):
                blk = slice(g * 512, (g + 1) * 512)
                b = 4 + oi % 2
                oi += 1
                pe_bank(b)
                for c in range(8):
                    PE(nc.tensor.matmul(PSB[b][:], lhsT=WO[:, c, dc * 128:(dc + 1) * 128], rhs=mixT[:, c, blk], start=(c == 0), stop=(c == 7)))
                DVE.wait(PE.sig())
                DVE(nc.vector.tensor_tensor(out=XT[:, dc, blk], in0=PSB[b][:], in1=XT[:, dc, blk], op=ALU.add))
                bank_free[b] = DVE.sig()

    barrier()
    for s in range(NSEQ):
        if s > 0:
            for E in ENGS:
                E.new_epoch()
        stage_load(s)
        barrier()
        if stop_after != "load":
            stage_ffn("ffn1")
            barrier()
        if stop_after not in ("load", "ffn1"):
            stage_mix()
            barrier()
            if stop_after not in ("mix", "mixcat"):
                stage_ffn("ffn2")
                barrier()
        stage_out(s)
        barrier()
    SP.wait(*[f for f in oring.free if f is not None])
    return nc


def kernel(**inputs):
    NCORES = 8
    NSEQ = 4
    nc = build(NSEQ)
    C = _consts()
    xs = np.ascontiguousarray(inputs["x"]).reshape(NCORES, NSEQ, S, D)
    base = {}
    for name, shp in WSPECS:
        base[name] = np.ascontiguousarray(np.asarray(inputs[name], dtype=np.float32).reshape(shp))
    for name, n in VSPECS:
        base[name] = np.ascontiguousarray(np.asarray(inputs[name], dtype=np.float32).reshape(n))
    for name, shp, dt in CONST_SPECS:
        base["c_" + name] = np.ascontiguousarray(C[name])
    in_maps = []
    for c in range(NCORES):
        m = dict(base)
        m["x"] = xs[c]
        in_maps.append(m)
    res = run_bass_kernel_spmd(nc, in_maps, core_ids=list(range(NCORES)))
    out = np.stack([r["y"] for r in res.results], axis=0).reshape(NCORES * NSEQ, S, D)
    return out.astype(np.float32)
```
